# Optimizing a Trainium2 kernel written in Bass

```python
import jax, jax.numpy as jnp
from jax import lax
import numpy as np

D_MODEL = 1024
BATCH = 8
SEQ = 8192
DEPTH = 4

HEAD_DIM = 64
N_FOX_HEADS = D_MODEL // (2 * HEAD_DIM)
N_RET_HEADS = D_MODEL // (2 * HEAD_DIM)
FOX_WIDTH = N_FOX_HEADS * HEAD_DIM
RET_WIDTH = N_RET_HEADS * HEAD_DIM
MIX_WIDTH = FOX_WIDTH + RET_WIDTH
D_FF = -(-8 * D_MODEL // (3 * 128)) * 128
CONV_WIDTH = 3
PLE_DIM = 256
Q_BLOCK = 128
RET_CHUNK = 128
ROPE_BASE = 10000.0
NORM_EPS = 1e-6
GN_EPS = 1e-5
IN_SIZES = (FOX_WIDTH, FOX_WIDTH, FOX_WIDTH, N_FOX_HEADS, RET_WIDTH, RET_WIDTH, RET_WIDTH, RET_WIDTH)
IN_WIDTH = sum(IN_SIZES)
SPLIT_POINTS = tuple(int(v) for v in np.cumsum(IN_SIZES)[:-1])

kernel_name = "hymba_fox_retnet_convffn_ple"


def rms_norm(x, w):
    xf = x.astype(jnp.float32)
    y = xf * lax.rsqrt(jnp.mean(xf * xf, axis=-1, keepdims=True) + NORM_EPS)
    return (y * w.astype(jnp.float32)).astype(x.dtype)


def split_heads(t, n_heads):
    b, s, _ = t.shape
    return t.reshape(b, s, n_heads, HEAD_DIM)


def rotary_tables(seq):
    inv_freq = ROPE_BASE ** (-jnp.arange(0, HEAD_DIM, 2, dtype=jnp.float32) / HEAD_DIM)
    ang = jnp.arange(seq, dtype=jnp.float32)[:, None] * inv_freq[None, :]
    return jnp.cos(ang), jnp.sin(ang)


def apply_rotary(t, cos, sin):
    half = HEAD_DIM // 2
    t1, t2 = t[..., :half], t[..., half:]
    c = cos[None, :, None, :]
    s = sin[None, :, None, :]
    return jnp.concatenate([t1 * c - t2 * s, t1 * s + t2 * c], axis=-1).astype(t.dtype)


def forgetting_attention(q, k, v, f_logit):
    b, s, h, d = q.shape
    q = q.transpose(0, 2, 1, 3)
    k = k.transpose(0, 2, 1, 3)
    v = v.transpose(0, 2, 1, 3)
    log_f = jax.nn.log_sigmoid(f_logit.astype(jnp.float32))
    cum = jnp.cumsum(log_f, axis=1).transpose(0, 2, 1)
    scale = HEAD_DIM ** -0.5
    kpos = jnp.arange(s)
    n_blocks = s // Q_BLOCK

    def one_block(bi):
        start = bi * Q_BLOCK
        qb = lax.dynamic_slice_in_dim(q, start, Q_BLOCK, axis=2)
        cq = lax.dynamic_slice_in_dim(cum, start, Q_BLOCK, axis=2)
        logits = jnp.einsum('bhqd,bhkd->bhqk', qb, k).astype(jnp.float32) * scale
        logits = logits + cq[..., :, None] - cum[..., None, :]
        qpos = start + jnp.arange(Q_BLOCK)
        logits = jnp.where(kpos[None, :] <= qpos[:, None], logits, -jnp.inf)
        probs = jax.nn.softmax(logits, axis=-1)
        return jnp.einsum('bhqk,bhkd->bhqd', probs.astype(v.dtype), v)

    out = lax.map(one_block, jnp.arange(n_blocks))
    return out.transpose(1, 0, 3, 2, 4).reshape(b, s, h * d)


def chunkwise_retention(q, k, v, log_gamma):
    b, s, h, d = q.shape
    n_chunks = s // RET_CHUNK

    def to_chunks(t):
        return t.reshape(b, n_chunks, RET_CHUNK, h, d).transpose(1, 0, 3, 2, 4)

    idx = jnp.arange(RET_CHUNK, dtype=jnp.float32)
    rel = idx[:, None] - idx[None, :]
    lg = log_gamma[:, None, None]
    intra_decay = jnp.where(rel >= 0, jnp.exp(lg * jnp.maximum(rel, 0.0)), 0.0)
    q_decay = jnp.exp(log_gamma[:, None] * (idx + 1.0))
    k_decay = jnp.exp(log_gamma[:, None] * (RET_CHUNK - 1.0 - idx))
    chunk_decay = jnp.exp(log_gamma * RET_CHUNK)

    def step(state, qkv):
        qc, kc, vc = qkv
        scores = jnp.einsum('bhid,bhjd->bhij', qc, kc) * intra_decay
        inner = jnp.einsum('bhij,bhje->bhie', scores, vc)
        cross = jnp.einsum('bhid,bhde->bhie', qc, state) * q_decay[:, :, None]
        new_state = state * chunk_decay[:, None, None] + jnp.einsum(
            'bhjd,bhje->bhde', kc * k_decay[:, :, None], vc)
        return new_state, inner + cross

    state0 = jnp.zeros((b, h, d, d), jnp.float32)
    _, out = lax.scan(step, state0, (to_chunks(q), to_chunks(k), to_chunks(v)))
    return out.transpose(1, 0, 3, 2, 4).reshape(b, s, h, d)


def head_group_norm(y):
    yf = y.astype(jnp.float32)
    mu = jnp.mean(yf, axis=-1, keepdims=True)
    var = jnp.mean(jnp.square(yf - mu), axis=-1, keepdims=True)
    return (yf - mu) * lax.rsqrt(var + GN_EPS)


def causal_depthwise_conv(a, w, bias):
    s = a.shape[1]
    ap = jnp.pad(a, ((0, 0), (CONV_WIDTH - 1, 0), (0, 0)))
    y = bias
    for j in range(CONV_WIDTH):
        y = y + ap[:, j:j + s, :] * w[j]
    return y


def gated_conv_ffn(u, w_up, conv_w, conv_b, w_down):
    up = u @ w_up
    a, g = up[..., :D_FF], up[..., D_FF:]
    a = causal_depthwise_conv(a, conv_w, conv_b)
    return (jax.nn.gelu(a, approximate=False) * g) @ w_down


def setup_inputs(seed: int = 0) -> dict:
    key = jax.random.key(seed)
    ks = jax.random.split(key, 16)
    nrm = jax.random.normal
    f32 = jnp.float32
    return {
        'x': nrm(ks[0], (BATCH, SEQ, D_MODEL), f32),
        'p': nrm(ks[1], (DEPTH, BATCH, SEQ, PLE_DIM), f32),
        'attn_norm_w': 1.0 + 0.05 * nrm(ks[2], (DEPTH, D_MODEL), f32),
        'w_in': nrm(ks[3], (DEPTH, D_MODEL, IN_WIDTH), f32) * D_MODEL ** -0.5,
        'forget_bias': 2.0 + 0.1 * nrm(ks[4], (DEPTH, N_FOX_HEADS), f32),
        'w_out': nrm(ks[5], (DEPTH, MIX_WIDTH, D_MODEL), f32) * MIX_WIDTH ** -0.5,
        'ffn_norm_w': 1.0 + 0.05 * nrm(ks[6], (DEPTH, D_MODEL), f32),
        'w_up': nrm(ks[7], (DEPTH, D_MODEL, 2 * D_FF), f32) * D_MODEL ** -0.5,
        'conv_w': nrm(ks[8], (DEPTH, CONV_WIDTH, D_FF), f32) * CONV_WIDTH ** -0.5,
        'conv_b': 0.01 * nrm(ks[9], (DEPTH, D_FF), f32),
        'w_down': nrm(ks[10], (DEPTH, D_FF, D_MODEL), f32) * D_FF ** -0.5,
        'ple_norm_w': 1.0 + 0.05 * nrm(ks[11], (DEPTH, D_MODEL), f32),
        'w_ple_gate': nrm(ks[12], (DEPTH, D_MODEL, D_MODEL), f32) * D_MODEL ** -0.5,
        'w_ple_proj': nrm(ks[13], (DEPTH, PLE_DIM, D_MODEL), f32) * PLE_DIM ** -0.5,
        'final_norm_w': 1.0 + 0.05 * nrm(ks[14], (D_MODEL,), f32),
    }


def reference(x, p, attn_norm_w, w_in, forget_bias, w_out, ffn_norm_w, w_up, conv_w, conv_b,
              w_down, ple_norm_w, w_ple_gate, w_ple_proj, final_norm_w):
    seq = x.shape[1]
    cos, sin = rotary_tables(seq)
    log_gamma = jnp.log1p(-jnp.exp2(-5.0 - jnp.arange(N_RET_HEADS, dtype=jnp.float32)))
    h = x
    for i in range(DEPTH):
        u = rms_norm(h, attn_norm_w[i])
        proj = u @ w_in[i]
        fq, fk, fv, f_logit, rq, rk, rv, rg = jnp.split(proj, SPLIT_POINTS, axis=-1)
        fox = forgetting_attention(split_heads(fq, N_FOX_HEADS), split_heads(fk, N_FOX_HEADS),
                                   split_heads(fv, N_FOX_HEADS), f_logit + forget_bias[i])
        rq_h = apply_rotary(split_heads(rq, N_RET_HEADS), cos, sin)
        rk_h = apply_rotary(split_heads(rk, N_RET_HEADS), cos, sin) * (HEAD_DIM ** -0.5)
        ret = chunkwise_retention(rq_h, rk_h, split_heads(rv, N_RET_HEADS), log_gamma)
        ret = head_group_norm(ret).reshape(ret.shape[0], seq, RET_WIDTH)
        ret = (jax.nn.silu(rg.astype(jnp.float32)) * ret).astype(x.dtype)
        mixed = jnp.concatenate([fox.astype(x.dtype), ret], axis=-1)
        h = h + mixed @ w_out[i]
        h = h + gated_conv_ffn(rms_norm(h, ffn_norm_w[i]), w_up[i], conv_w[i], conv_b[i], w_down[i])
        gate = jax.nn.sigmoid(rms_norm(h, ple_norm_w[i]) @ w_ple_gate[i])
        h = h + gate * (p[i] @ w_ple_proj[i])
    return rms_norm(h, final_norm_w)
```

```python
import contextlib
import os
import numpy as np
import ml_dtypes
import concourse.bass as bass
import concourse.mybir as mybir
from concourse.bass_utils import run_bass_kernel_spmd

F32 = mybir.dt.float32
BF16 = mybir.dt.bfloat16
AF = mybir.ActivationFunctionType
ALU = mybir.AluOpType

D = 1024
HD = 64
NH = 8
DFF = 2816
NFC = DFF // 128
PLE = 256
INW = 3592
C_FQ, C_FK, C_FV, C_FL, C_RQ, C_RK, C_RV, C_RG = 0, 512, 1024, 1536, 1544, 2056, 2568, 3080
NORM_EPS = 1e-6
GN_EPS = 1e-5
NEG = -30000.0


class Buf:
    __slots__ = ("name", "w", "r")

    def __init__(self, name):
        self.name = name
        self.w = {}
        self.r = {}


def _key(tok):
    return tok[1] if tok[0] == "e" else (tok[1], tok[2])


def _put(d, tok):
    k = _key(tok)
    o = d.get(k)
    if o is None or o[-1] < tok[-1]:
        d[k] = tok


class Sched:
    def __init__(self, nc, stack):
        self.nc = nc
        self.eng = {"pe": nc.tensor, "act": nc.scalar, "dve": nc.vector, "pool": nc.gpsimd, "sp": nc.sync}
        self.sem = {k: stack.enter_context(nc.semaphore("s_" + k)) for k in self.eng}
        self.cnt = {k: 0 for k in self.eng}
        self.waited = {k: {} for k in self.eng}
        self.dq = {}
        for q, n in (("sp", 16), ("pool", 24), ("act", 2)):
            self.dq[q] = [[stack.enter_context(nc.semaphore("d_%s%d" % (q, i))), 0] for i in range(n)]
        self.dqi = {q: 0 for q in self.dq}
        self.nwait = 0
        self.nops = 0

    def _wait(self, e, tok):
        if tok[0] == "e":
            p, v = tok[1], tok[2]
            if p == e and e == "pe":
                return
            key = p
            sem = self.sem[p]
        else:
            q, i, v = tok[1], tok[2], tok[3]
            key = (q, i)
            sem = self.dq[q][i][0]
        if self.waited[e].get(key, 0) >= v:
            return
        self.waited[e][key] = v
        self.eng[e].wait_ge(sem, v)
        self.nwait += 1

    def _deps(self, e, r, w, pw):
        toks = []
        for b in r:
            toks += list(b.w.values())
        for b in w:
            toks += list(b.w.values())
            toks += list(b.r.values())
        for b in pw:
            toks += list(b.r.values())
        for t in toks:
            self._wait(e, t)

    def _upd(self, tok, r, w, pw):
        for b in r:
            _put(b.r, tok)
        for b in w:
            b.w = {_key(tok): tok}
            b.r = {}
        for b in pw:
            if b.r:
                b.w = {_key(tok): tok}
                b.r = {}
            else:
                _put(b.w, tok)

    def op(self, e, fn, r=(), w=(), pw=(), signal=True):
        self._deps(e, r, w, pw)
        inst = fn()
        self.nops += 1
        if signal:
            self.cnt[e] += 1
            inst.then_inc(self.sem[e], 1)
            tok = ("e", e, self.cnt[e])
        else:
            tok = ("e", e, self.cnt[e] + 1)
        self._upd(tok, r, w, pw)
        return tok

    def dma(self, q, out, in_, r=(), w=(), pw=(), **kw):
        i = self.dqi[q]
        self.dqi[q] = (i + 1) % len(self.dq[q])
        slot = self.dq[q][i]
        if slot[1] > 0:
            self._wait(q, ("d", q, i, slot[1]))
        self._deps(q, r, w, pw)
        inst = self.eng[q].dma_start(out=out, in_=in_, **kw)
        self.nops += 1
        slot[1] += 16
        inst.then_inc(slot[0], 16)
        tok = ("d", q, i, slot[1])
        self._upd(tok, r, w, pw)
        return tok

    def barrier(self, scratch):
        m = "dve"
        for p in self.eng:
            if p != m and self.cnt[p] > 0:
                self._wait(m, ("e", p, self.cnt[p]))
        for q in self.dq:
            for i, (s, c) in enumerate(self.dq[q]):
                if c > 0:
                    self._wait(m, ("d", q, i, c))
        if self.cnt[m] > 0:
            self._wait(m, ("e", m, self.cnt[m]))
        inst = self.nc.vector.memset(scratch, 0.0)
        self.cnt[m] += 1
        inst.then_inc(self.sem[m], 1)
        tok = ("e", m, self.cnt[m])
        for e in self.eng:
            if e != m:
                self._wait(e, tok)
            for p in self.eng:
                self.waited[e][p] = max(self.waited[e].get(p, 0), self.cnt[p] if p != m or e != m else self.cnt[p])
            for q in self.dq:
                for i, (s, c) in enumerate(self.dq[q]):
                    self.waited[e][(q, i)] = max(self.waited[e].get((q, i), 0), c)


def sb_ap(t, offset, pairs):
    return bass.AP(t, offset, [list(p) for p in pairs])


def build(S, depth, do_final=True, phases="12345"):
    NB = S // 128
    NT = S // 512
    nc = bass.Bass("TRN2", target_bir_lowering=False)

    def din(name, shape, dt=F32):
        return nc.dram_tensor(name, list(shape), dt, kind="ExternalInput")

    def dscr(name, shape, dt):
        return nc.dram_tensor(name, list(shape), dt, kind="Internal")

    x = din("x", [S, D])
    p_in = din("p", [depth, S, PLE])
    attn_norm_w = din("attn_norm_w", [depth, D])
    w_in = din("w_in", [depth, D, INW])
    forget_bias = din("forget_bias", [depth, NH])
    w_out = din("w_out", [depth, D, D])
    ffn_norm_w = din("ffn_norm_w", [depth, D])
    w_up = din("w_up", [depth, D, 2 * DFF])
    conv_w = din("conv_w", [depth, 3, DFF])
    conv_b = din("conv_b", [depth, DFF])
    w_down = din("w_down", [depth, DFF, D])
    ple_norm_w = din("ple_norm_w", [depth, D])
    w_ple_gate = din("w_ple_gate", [depth, D, D])
    w_ple_proj = din("w_ple_proj", [depth, PLE, D])
    final_norm_w = din("final_norm_w", [D])
    c_cosq = din("c_cosq", [128, S])
    c_sinq = din("c_sinq", [128, S])
    c_cosk = din("c_cosk", [128, S])
    c_sink = din("c_sink", [128, S])
    c_identb = din("c_identb", [128, 128], BF16)
    c_identf = din("c_identf", [128, 128])
    c_pswap = din("c_pswap", [128, 128], BF16)
    c_negmask = din("c_negmask", [128, 128], BF16)
    c_DT = din("c_DT", [128, NH * 128])
    c_qdec = din("c_qdec", [128, NH])
    c_kdec = din("c_kdec", [128, NH])
    c_cdec = din("c_cdec", [128, 4 * 64])

    out = nc.dram_tensor("out", [S, D], F32, kind="ExternalOutput")

    hA = dscr("hA", [S, D], F32)
    hB = dscr("hB", [S, D], F32)
    fqT_d = dscr("fqT_d", [NH, 65, S], BF16)
    fkT_d = dscr("fkT_d", [NH, 64, S], BF16)
    fv_d = dscr("fv_d", [S, 512], BF16)
    rqT_d = dscr("rqT_d", [512, S], BF16)
    rkT_d = dscr("rkT_d", [512, S], BF16)
    rv_d = dscr("rv_d", [S, 512], BF16)
    rg_d = dscr("rg_d", [S, 512], BF16)
    mixed_d = dscr("mixed_d", [S, 1024], BF16)
    act_d = dscr("act_d", [DFF, S], BF16)

    with contextlib.ExitStack() as gst:
        G = gst.enter_context
        sc = Sched(nc, gst)
        op, dma = sc.op, sc.dma
        V, A, P, T = nc.vector, nc.scalar, nc.gpsimd, nc.tensor

        uid = {"n": 0}

        def sbt(st, name, shape, dt):
            uid["n"] += 1
            return st.enter_context(nc.sbuf_tensor("%s_%d" % (name, uid["n"]), list(shape), dt))

        def pst(st, name, shape, dt):
            uid["n"] += 1
            return st.enter_context(nc.psum_tensor("%s_%d" % (name, uid["n"]), list(shape), dt))

        identb = sbt(gst, "identb", [128, 128], BF16)
        identf = sbt(gst, "identf", [128, 128], F32)
        pswap = sbt(gst, "pswap", [128, 128], BF16)
        negmask = sbt(gst, "negmask", [128, 128], BF16)
        epsn = sbt(gst, "epsn", [128, 1], F32)
        epsg = sbt(gst, "epsg", [128, 1], F32)
        barr = sbt(gst, "barr", [128, 1], F32)
        negcum = sbt(gst, "negcum", [128, NB, NH], F32)
        B_const = Buf("const")
        B_negcum = Buf("negcum")
        dma("sp", identb[:], c_identb.ap(), w=[B_const])
        dma("sp", identf[:], c_identf.ap(), pw=[B_const])
        dma("sp", pswap[:], c_pswap.ap(), pw=[B_const])
        dma("sp", negmask[:], c_negmask.ap(), pw=[B_const])
        op("pool", lambda: P.memset(epsn[:], NORM_EPS), pw=[B_const])
        op("pool", lambda: P.memset(epsg[:], GN_EPS), pw=[B_const])
        sc.barrier(barr[:])

        rr = {"i": 0}

        def evac_engine():
            rr["i"] += 1
            return "act" if rr["i"] % 2 else "dve"

        def copy_on(e, out_ap, in_ap, r, w=(), pw=(), scale=None):
            if e == "act":
                if scale is None:
                    return op("act", lambda: A.copy(out=out_ap, in_=in_ap), r=r, w=w, pw=pw)
                return op("act", lambda: A.mul(out=out_ap, in_=in_ap, mul=scale), r=r, w=w, pw=pw)
            if e == "dve":
                if scale is None:
                    return op("dve", lambda: V.tensor_copy(out=out_ap, in_=in_ap), r=r, w=w, pw=pw)
                return op("dve", lambda: V.tensor_scalar(out=out_ap, in0=in_ap, scalar1=scale, scalar2=None,
                                                         op0=ALU.mult), r=r, w=w, pw=pw)
            if scale is None:
                return op("pool", lambda: P.tensor_copy(out=out_ap, in_=in_ap), r=r, w=w, pw=pw)
            return op("pool", lambda: P.tensor_scalar(out=out_ap, in0=in_ap, scalar1=scale, scalar2=None,
                                                      op0=ALU.mult), r=r, w=w, pw=pw)

        def load_w(dst, k, src_rows_ap, ncols, B, piece):
            c0 = 0
            while c0 < ncols:
                c1 = min(ncols, c0 + piece)
                dma("pool", dst[:, k, c0:c1], src_rows_ap[:, c0:c1], pw=[B])
                c0 = c1

        def bcast_row(dst, src_t, off, n, B):
            src = bass.AP(src_t, off, [[0, 128], [1, n]])
            dma("pool", dst, src, w=[B])

        class Normer:
            def __init__(self, st, tag, wn, B_wn):
                self.wn, self.B_wn = wn, B_wn
                self.junk = sbt(st, tag + "junk", [128, D], BF16)
                self.B_junk = Buf("junk")
                self.ss = [sbt(st, tag + "ss%d" % i, [128, 1], F32) for i in range(3)]
                self.sd = [sbt(st, tag + "sd%d" % i, [128, 1], F32) for i in range(3)]
                self.rs = [sbt(st, tag + "rs%d" % i, [128, 1], F32) for i in range(3)]
                self.B_ss = [Buf("ss") for _ in range(3)]
                self.B_sd = [Buf("sd") for _ in range(3)]
                self.B_rs = [Buf("rs") for _ in range(3)]
                self.ub = [sbt(st, tag + "ub%d" % i, [128, D], BF16) for i in range(2)]
                self.B_ub = [Buf("ub") for _ in range(2)]
                self.ptr = [pst(st, tag + "ptr%d" % i, [128, 8, 128], BF16) for i in range(2)]
                self.B_ptr = [Buf("ptr0"), Buf("ptr1")]
                self.i = 0

            def rstd(self, hb, B_hb, i=None):
                if i is None:
                    i = self.i % 2
                ss, sd, rs = self.ss[i], self.sd[i], self.rs[i]
                op("act", lambda: A.activation(out=self.junk[:], in_=hb, func=AF.Square, accum_out=ss[:]),
                   r=[B_hb], w=[self.B_junk, self.B_ss[i]])
                op("act", lambda: A.activation(out=sd[:], in_=ss[:], func=AF.Sqrt, bias=epsn[:], scale=1.0 / D),
                   r=[self.B_ss[i], B_const], w=[self.B_sd[i]])
                op("dve", lambda: V.reciprocal(out=rs[:], in_=sd[:]), r=[self.B_sd[i]], w=[self.B_rs[i]])
                return rs, self.B_rs[i]

            def run_a(self, hb, B_hb):
                i = self.i % 2
                rs, B_rs = self.rstd(hb, B_hb)
                ub = self.ub[i]
                op("dve", lambda: V.scalar_tensor_tensor(out=ub[:], in0=hb, scalar=rs[:], in1=self.wn[:],
                                                         op0=ALU.mult, op1=ALU.mult),
                   r=[B_hb, B_rs, self.B_wn], w=[self.B_ub[i]])
                self.i += 1
                return ub, self.B_ub[i]

            def run(self, hb, B_hb, uT, B_uT, c0):
                ub, B_ub = self.run_a(hb, B_hb)
                self.transpose8(ub, B_ub, uT, B_uT, c0, 8)

            def transpose8(self, ub, B_ub, uT, B_uT, c0, nk):
                for j in range((nk + 3) // 4):
                    n = min(4, nk - 4 * j)
                    for kk in range(n):
                        k = 4 * j + kk
                        op("pe", lambda k=k, kk=kk, j=j: T.transpose(out=self.ptr[j][:, kk, :],
                                                                     in_=ub[:, k * 128:(k + 1) * 128],
                                                                     identity=identb[:]),
                           r=[B_ub, B_const], pw=[self.B_ptr[j]])
                    copy_on(evac_engine(), uT[:, 4 * j:4 * j + n, c0:c0 + 128], self.ptr[j][:, 0:n, :],
                            r=[self.B_ptr[j]], pw=[B_uT])

        def phase1(l, hsrc):
            with contextlib.ExitStack() as st:
                win = sbt(st, "win", [128, 8, INW], BF16)
                B_win = Buf("win")
                for k in range(8):
                    load_w(win, k, w_in.ap()[l, k * 128:(k + 1) * 128, :], INW, B_win, 1796)
                wn = sbt(st, "wn1", [128, D], F32)
                B_wn = Buf("wn")
                bcast_row(wn[:], attn_norm_w, l * D, D, B_wn)
                fb = sbt(st, "fb", [NH, 1], F32)
                nfb = sbt(st, "nfb", [NH, 1], F32)
                B_fb, B_nfb = Buf("fb"), Buf("nfb")
                dma("sp", fb[:], bass.AP(forget_bias, l * NH, [[1, NH], [1, 1]]), w=[B_fb])
                op("dve", lambda: V.tensor_scalar(out=nfb[:], in0=fb[:], scalar1=-1.0, scalar2=None, op0=ALU.mult),
                   r=[B_fb], w=[B_nfb])
                nm = Normer(st, "n1", wn, B_wn)
                hb = [sbt(st, "hb%d" % i, [128, D], F32) for i in range(4)]
                B_hb = [Buf("hb") for _ in range(4)]
                uT = [sbt(st, "uT%d" % i, [128, 8, 512], BF16) for i in range(2)]
                B_uT = [Buf("uT") for _ in range(2)]
                tabs = [[sbt(st, "tab%d_%d" % (i, j), [128, 512], F32) for j in range(4)] for i in range(2)]
                B_tabs = [Buf("tabs") for _ in range(2)]
                pf = [pst(st, "pf%d" % i, [128, 512], F32) for i in range(2)]
                B_pf = [Buf("pf") for _ in range(2)]
                pt = [pst(st, "pt%d" % i, [128, 512], F32) for i in range(2)]
                B_pt = [Buf("pt") for _ in range(2)]
                pr = pst(st, "pr", [128, 512], F32)
                B_pr = Buf("pr")
                plnt = pst(st, "plnt", [128, 512], F32)
                pl = plnt[0:NH, :]
                pnt = plnt[:, 0:4 * NH].rearrange("p (j h) -> p j h", j=4)
                B_pl = Buf("plnt")
                B_pnt = B_pl
                NST = 4
                stg = [sbt(st, "stg%d" % i, [128, 512], BF16) for i in range(NST)]
                B_stg = [Buf("stg") for _ in range(NST)]
                qb = [sbt(st, "qb%d" % i, [128, 512], BF16) for i in range(2)]
                B_qb = [Buf("qb") for _ in range(2)]
                t1 = [sbt(st, "t1_%d" % i, [128, 512], F32) for i in range(2)]
                t2 = [sbt(st, "t2_%d" % i, [128, 512], F32) for i in range(2)]
                B_t1 = [Buf("t1") for _ in range(2)]
                B_t2 = [Buf("t2") for _ in range(2)]
                ef = sbt(st, "ef", [NH, 512], F32)
                lf = sbt(st, "lf", [NH, 512], F32)
                B_ef, B_lf = Buf("ef"), Buf("lf")
                ones8 = sbt(st, "ones8", [NH, 512], F32)
                B_ones8 = Buf("ones8")
                op("pool", lambda: P.memset(ones8[:], 1.0), w=[B_ones8])
                ncT = [sbt(st, "ncT%d" % i, [NH, 512], F32) for i in range(2)]
                B_ncT = [Buf("ncT") for _ in range(2)]
                cb = sbt(st, "cb", [NH, 512], BF16)
                B_cb = Buf("cb")
                cnt = {"pf": 0, "pt": 0, "stg": 0, "rot": 0}

                def norm_gen(t):
                    s = t % 2
                    tb = tabs[s]
                    for j, c in enumerate((c_cosq, c_sinq, c_cosk, c_sink)):
                        dma("sp", tb[j][:], c.ap()[:, t * 512:(t + 1) * 512], pw=[B_tabs[s]])
                    for sub in range(4):
                        blk = t * 4 + sub
                        dma("sp", hb[sub][:], hsrc[blk * 128:(blk + 1) * 128, :], w=[B_hb[sub]])
                    for sub in range(4):
                        i = sub
                        ub_, B_ub_ = nm.run_a(hb[i][:], B_hb[i])
                        yield
                        nm.transpose8(ub_, B_ub_, uT[s], B_uT[s], sub * 128, 8)
                        yield

                def norm_tile(t):
                    for _ in norm_gen(t):
                        pass

                def fmajor(t, c0, M=128):
                    s = t % 2
                    i = cnt["pf"] % 2
                    cnt["pf"] += 1
                    for k in range(8):
                        op("pe", lambda k=k: T.matmul(pf[i][0:M, :], lhsT=win[:, k, c0:c0 + M], rhs=uT[s][:, k, :],
                                                      start=(k == 0), stop=(k == 7)),
                           r=[B_win, B_uT[s]], pw=[B_pf[i]], signal=(k == 7))
                    return pf[i], B_pf[i]

                def next_stg():
                    i = cnt["stg"] % NST
                    cnt["stg"] += 1
                    return stg[i], B_stg[i]

                def mm_tile(t, lvl=9):
                    s = t % 2
                    tok = slice(t * 512, (t + 1) * 512)
                    gen = norm_gen(t + 1) if t + 1 < NT else None
                    pc = {"n": 0}

                    def pull():
                        pc["n"] += 1
                        if gen is not None and pc["n"] % 3 == 0:
                            next(gen, None)
                    for grp, c0, dst, scale in (("fq", C_FQ, fqT_d, 0.125), ("fk", C_FK, fkT_d, None)):
                        for cc in range(4):
                            ps, B_ps = fmajor(t, c0 + cc * 128)
                            sg, B_sg = next_stg()
                            copy_on(evac_engine(), sg[:], ps[:], r=[B_ps], w=[B_sg], scale=scale)
                            for hh in range(2):
                                dma("pool", dst.ap()[2 * cc + hh, 0:64, tok], sg[hh * 64:(hh + 1) * 64, :], r=[B_sg])
                            pull()
                    if lvl < 3:
                        return
                    i = cnt["pf"] % 2
                    cnt["pf"] += 1
                    for k in range(8):
                        op("pe", lambda k=k: T.matmul(pl, lhsT=win[:, k, C_FL:C_FL + NH], rhs=uT[s][:, k, :],
                                                      start=(k == 0), stop=(k == 7)),
                           r=[B_win, B_uT[s]], w=([B_pl] if k == 0 else []), pw=([] if k == 0 else [B_pl]),
                           signal=(k == 7))
                    op("act", lambda: A.activation(out=ef[:], in_=pl, func=AF.Exp, bias=nfb[:], scale=-1.0),
                       r=[B_pl, B_nfb], w=[B_ef])
                    op("act", lambda: A.activation(out=lf[:], in_=ef[:], func=AF.Ln, bias=1.0, scale=1.0),
                       r=[B_ef], w=[B_lf])
                    ci = t % 2
                    init = 0.0 if t == 0 else ncT[1 - ci][:, 511:512]
                    op("dve", lambda: V.tensor_tensor_scan(out=ncT[ci][:], data0=ones8[:], data1=lf[:], initial=init,
                                                           op0=ALU.mult, op1=ALU.add),
                       r=[B_lf, B_ones8, B_ncT[1 - ci]], w=[B_ncT[ci]])
                    op("dve", lambda: V.tensor_scalar(out=cb[:], in0=ncT[ci][:], scalar1=-1.0, scalar2=None,
                                                      op0=ALU.mult), r=[B_ncT[ci]], w=[B_cb])
                    dma("pool", fqT_d.ap()[:, 64, tok], cb[:], r=[B_cb])
                    for j in range(4):
                        op("pe", lambda j=j: T.transpose(out=pnt[:, j, :], in_=ncT[ci][:, j * 128:(j + 1) * 128],
                                                         identity=identf[0:NH, 0:NH]),
                           r=[B_ncT[ci], B_const], w=([B_pnt] if j == 0 else []), pw=([] if j == 0 else [B_pnt]))
                    op("dve", lambda: V.tensor_copy(out=negcum[:, t * 4:(t + 1) * 4, :], in_=pnt),
                       r=[B_pnt], pw=[B_negcum])
                    if lvl < 4:
                        return
                    deferred = {"f": None}
                    for grp, c0, dst, tj in (("rq", C_RQ, rqT_d, 0), ("rk", C_RK, rkT_d, 2)):
                        for cc in range(4):
                            ps, B_ps = fmajor(t, c0 + cc * 128)
                            ri = cnt["rot"] % 2
                            cnt["rot"] += 1
                            op("act", lambda: A.copy(out=qb[ri][:], in_=ps[:]), r=[B_ps], w=[B_qb[ri]])
                            op("dve", lambda: V.tensor_tensor(out=t1[ri][:], in0=ps[:], in1=tabs[s][tj][:],
                                                              op=ALU.mult), r=[B_ps, B_tabs[s], B_qb[ri]], w=[B_t1[ri]])
                            if deferred["f"] is not None:
                                deferred["f"]()

                            def rot_rest(ri=ri, tj=tj, dst=dst, cc=cc):
                                op("pe", lambda: T.matmul(pr[:], lhsT=pswap[:], rhs=qb[ri][:], start=True, stop=True),
                                   r=[B_qb[ri], B_const], w=[B_pr])
                                op("dve", lambda: V.tensor_tensor(out=t2[ri][:], in0=pr[:], in1=tabs[s][tj + 1][:],
                                                                  op=ALU.mult), r=[B_pr, B_tabs[s]], w=[B_t2[ri]])
                                sg, B_sg = next_stg()
                                op("dve", lambda: V.tensor_tensor(out=sg[:], in0=t1[ri][:], in1=t2[ri][:], op=ALU.add),
                                   r=[B_t1[ri], B_t2[ri]], w=[B_sg])
                                dma("pool", dst.ap()[cc * 128:(cc + 1) * 128, tok], sg[:], r=[B_sg])
                                deferred["f"] = None
                            deferred["f"] = rot_rest
                            pull()
                    if lvl < 5:
                        return
                    for sub in range(4):
                        rows = slice(t * 512 + sub * 128, t * 512 + (sub + 1) * 128)
                        for c0, dst in ((C_FV, fv_d), (C_RV, rv_d), (C_RG, rg_d)):
                            i = cnt["pt"] % 2
                            cnt["pt"] += 1
                            for k in range(8):
                                op("pe", lambda k=k: T.matmul(pt[i][:], lhsT=uT[s][:, k, sub * 128:(sub + 1) * 128],
                                                              rhs=win[:, k, c0:c0 + 512], start=(k == 0), stop=(k == 7)),
                                   r=[B_win, B_uT[s]], pw=[B_pt[i]], signal=(k == 7))
                            if deferred["f"] is not None:
                                deferred["f"]()
                            sg, B_sg = next_stg()
                            copy_on(evac_engine(), sg[:], pt[i][:], r=[B_pt[i]], w=[B_sg])
                            dma("pool", dst.ap()[rows, :], sg[:], r=[B_sg])
                            pull()
                    if gen is not None:
                        for _ in gen:
                            pass

                norm_tile(0)
                for t in range(NT):
                    mm_tile(t)
                sc.barrier(barr[:])

        def phase2(l):
            with contextlib.ExitStack() as st:
                KT = [sbt(st, "KT%d" % i, [65, S], BF16) for i in range(2)]
                QT = [sbt(st, "QT%d" % i, [65, S], BF16) for i in range(2)]
                VA = [sbt(st, "VA%d" % i, [128, NB, 65], BF16) for i in range(2)]
                B_KT = [Buf("KT") for _ in range(2)]
                B_QT = [Buf("QT") for _ in range(2)]
                B_VA = [Buf("VA") for _ in range(2)]
                for i in range(2):
                    op("pool", lambda i=i: P.memset(KT[i][64:65, :], 1.0), pw=[B_KT[i]])
                    op("pool", lambda i=i: P.memset(VA[i][:, :, 64:65], 1.0), pw=[B_VA[i]])
                NPS = 3
                ps = [pst(st, "ps%d" % i, [128, 512], F32) for i in range(NPS)]
                B_ps = [Buf("ps") for _ in range(NPS)]
                poT = [pst(st, "poT%d" % i, [128, 512], F32) for i in range(2)]
                B_poT = [Buf("poT") for _ in range(2)]
                pfin = pst(st, "pfin", [128, 4, 128], F32)
                B_pfin = Buf("pfin")
                NPT = 4
                PT = [sbt(st, "PT%d" % i, [128, 512], BF16) for i in range(NPT)]
                B_PT = [Buf("PT") for _ in range(NPT)]
                oT = [sbt(st, "oT%d" % i, [65, 512], F32) for i in range(2)]
                B_oT = [Buf("oT") for _ in range(2)]
                rsum = [sbt(st, "rsum%d" % i, [128, 4], F32) for i in range(2)]
                B_rsum = [Buf("rsum") for _ in range(2)]
                fo = [sbt(st, "fo%d" % i, [128, 4, 64], BF16) for i in range(2)]
                B_fo = [Buf("fo") for _ in range(2)]
                cnt = {"ps": 0, "fo": 0}

                def load_head(h):
                    i = h % 2
                    dma("sp", KT[i][0:64, :], fkT_d.ap()[h], pw=[B_KT[i]], r=[])
                    dma("sp", QT[i][0:65, :], fqT_d.ap()[h], w=[B_QT[i]])
                    src = fv_d.ap()[:, h * 64:(h + 1) * 64].rearrange("(n p) e -> p n e", p=128)
                    dma("sp", VA[i][:, :, 0:64], src, pw=[B_VA[i]])

                def emit_qk(stp):
                    h, qt, kb = stp
                    i = h % 2
                    q0 = qt * 512
                    di = kb - 4 * qt
                    j0 = max(di, 0)
                    N = 512 - 128 * j0
                    pi = cnt["ps"] % NPS
                    cnt["ps"] += 1
                    diag = di >= 0
                    op("pe", lambda: T.matmul(ps[pi][:, 0:N], lhsT=KT[i][0:65, kb * 128:(kb + 1) * 128],
                                              rhs=QT[i][0:65, q0 + 128 * j0:q0 + 512], start=True, stop=not diag),
                       r=[B_KT[i], B_QT[i]], w=[B_ps[pi]], signal=not diag)
                    if diag:
                        op("pe", lambda: T.matmul(ps[pi][:, 0:128], lhsT=identb[:], rhs=negmask[:],
                                                  start=False, stop=True), r=[B_const], pw=[B_ps[pi]])
                    return pi

                def emit_rest(stp, pi, fi):
                    h, qt, kb = stp
                    i = h % 2
                    nkb = 4 * qt + 4
                    j0 = max(kb - 4 * qt, 0)
                    N = 512 - 128 * j0
                    ti = cnt["pt"] % NPT
                    cnt["pt"] += 1
                    op("act", lambda: A.activation(out=PT[ti][:, 0:N], in_=ps[pi][:, 0:N], func=AF.Exp,
                                                   bias=negcum[:, kb, h:h + 1], scale=1.0),
                       r=[B_ps[pi], B_negcum], w=[B_PT[ti]])
                    op("pe", lambda: T.matmul(poT[fi][0:65, 128 * j0:512], lhsT=VA[i][:, kb, :],
                                              rhs=PT[ti][:, 0:N], start=(kb == 0), stop=(kb == nkb - 1)),
                       r=[B_PT[ti], B_VA[i]], pw=[B_poT[fi]])

                def fin_a(fi):
                    op("dve", lambda: V.tensor_copy(out=oT[fi][:], in_=poT[fi][0:65, :]), r=[B_poT[fi]], w=[B_oT[fi]])

                def fin_b(h, qt, fi):
                    q0 = qt * 512
                    for j in range(4):
                        op("pe", lambda j=j: T.transpose(out=pfin[:, j, 0:65], in_=oT[fi][0:65, j * 128:(j + 1) * 128],
                                                         identity=identf[0:65, 0:65]),
                           r=[B_oT[fi], B_const], w=([B_pfin] if j == 0 else []), pw=([] if j == 0 else [B_pfin]))
                    op("dve", lambda: V.reciprocal(out=rsum[fi][:].rearrange("p (j o) -> p j o", o=1),
                                                   in_=pfin[:, :, 64:65]), r=[B_pfin], w=[B_rsum[fi]])
                    op("dve", lambda: V.tensor_tensor(out=fo[fi][:], in0=pfin[:, :, 0:64],
                                                      in1=sb_ap(rsum[fi], 0, [[4, 128], [1, 4], [0, 64]]),
                                                      op=ALU.mult),
                       r=[B_pfin, B_rsum[fi]], w=[B_fo[fi]])
                    dst = mixed_d.ap()[q0:q0 + 512, h * 64:(h + 1) * 64].rearrange("(j p) e -> p j e", p=128)
                    dma("pool", dst, fo[fi][:], r=[B_fo[fi]])

                cnt["pt"] = 0
                steps = [(h, qt, kb) for h in range(NH) for qt in range(NT) for kb in range(4 * qt + 4)]
                LOOK = 2
                load_head(0)
                pis = [emit_qk(steps[n]) for n in range(min(LOOK, len(steps)))]
                pending = []
                tile_no = 0
                for n, stp in enumerate(steps):
                    h, qt, kb = stp
                    if qt == 0 and kb == 0 and h + 1 < NH:
                        load_head(h + 1)
                    if n + LOOK < len(steps):
                        pis.append(emit_qk(steps[n + LOOK]))
                    fi = tile_no % 2
                    emit_rest(stp, pis[n], fi)
                    for pnd in pending:
                        pnd[0] -= 1
                    while pending and pending[0][0] <= 0:
                        _, ph, pq, pf_ = pending.pop(0)
                        fin_b(ph, pq, pf_)
                    if kb == 4 * qt + 3:
                        fin_a(fi)
                        pending.append([2, h, qt, fi])
                        tile_no += 1
                for _, ph, pq, pf_ in pending:
                    fin_b(ph, pq, pf_)
                sc.barrier(barr[:])

        def phase3(l):
            with contextlib.ExitStack() as st:
                DT = sbt(st, "DT", [128, NH, 128], F32)
                qdec = sbt(st, "qdec", [128, NH], F32)
                kdec = sbt(st, "kdec", [128, NH], F32)
                cdec = sbt(st, "cdec", [128, 4, 64], F32)
                B_c3 = Buf("c3")
                dma("sp", DT[:], c_DT.ap().rearrange("p (h i) -> p h i", h=NH), w=[B_c3])
                dma("sp", qdec[:], c_qdec.ap(), pw=[B_c3])
                dma("sp", kdec[:], c_kdec.ap(), pw=[B_c3])
                dma("sp", cdec[:], c_cdec.ap().rearrange("p (a e) -> p a e", a=4), pw=[B_c3])
                stf = sbt(st, "stf", [128, 4, 64], F32)
                stb = sbt(st, "stb", [128, 4, 64], BF16)
                B_stf, B_stb = Buf("stf"), Buf("stb")
                op("pool", lambda: P.memset(stf[:], 0.0), w=[B_stf])
                op("pool", lambda: P.memset(stb[:], 0.0), w=[B_stb])
                QT = [sbt(st, "rQT%d" % i, [128, 4, 128], BF16) for i in range(2)]
                KT = [sbt(st, "rKT%d" % i, [128, 4, 128], BF16) for i in range(2)]
                Vt = [sbt(st, "rV%d" % i, [128, 512], BF16) for i in range(2)]
                Gt = [sbt(st, "rG%d" % i, [128, 512], BF16) for i in range(2)]
                B_in = [Buf("r_in") for _ in range(2)]
                Kt = [sbt(st, "rKt%d" % i, [128, 512], BF16) for i in range(2)]
                B_Kt = [Buf("Kt") for _ in range(2)]
                Vd = [sbt(st, "rVd%d" % i, [128, NH, 64], BF16) for i in range(2)]
                B_Vd = [Buf("Vd") for _ in range(2)]
                AT = [sbt(st, "rAT%d" % i, [128, 128], BF16) for i in range(3)]
                B_AT = [Buf("AT") for _ in range(3)]
                pkt_t = pst(st, "pkt", [128, 8, 128], BF16)
                pkt = pkt_t[:, 0:4, :]
                B_pkt = Buf("pkt")
                psc_t = [pst(st, "psc%d" % i, [128, 512], F32) for i in range(2)]
                psc = [t_[:, 0:128] for t_ in psc_t]
                B_psc = [Buf("psc") for _ in range(2)]
                pout2 = [pst(st, "pout%d" % i, [128, NH, 64], F32) for i in range(2)]
                B_pout2 = [Buf("pout") for _ in range(2)]
                pstate_t = pst(st, "pstate", [128, 8, 64], F32)
                pstate = pstate_t[:, 0:4, :]
                B_pstate = Buf("pstate")
                raw = sbt(st, "raw", [128, NH, 64], F32)
                cen = sbt(st, "cen", [128, NH, 64], F32)
                sq = sbt(st, "sq", [128, NH, 64], F32)
                nrm = sbt(st, "nrm", [128, NH, 64], F32)
                sg = sbt(st, "sg", [128, 512], F32)
                B_raw, B_cen, B_sq, B_nrm, B_sg = Buf("raw"), Buf("cen"), Buf("sq"), Buf("nrm"), Buf("sg")
                mean = sbt(st, "mean", [128, NH], F32)
                var = sbt(st, "var", [128, NH], F32)
                sdv = sbt(st, "sdv", [128, NH], F32)
                rstd = sbt(st, "rstd", [128, NH], F32)
                B_mean, B_var, B_sdv, B_rstd = Buf("mean"), Buf("var"), Buf("sdv"), Buf("rstd")
                res = [sbt(st, "res%d" % i, [128, 512], BF16) for i in range(2)]
                B_res = [Buf("res") for _ in range(2)]
                cnt = {"sc": 0, "at": 0}

                def bc3(t, n):
                    return sb_ap(t, 0, [[NH, 128], [1, NH], [0, n]])

                def load(c):
                    i = c % 2
                    tok = slice(c * 128, (c + 1) * 128)
                    dma("sp", QT[i][:], rqT_d.ap()[:, tok].rearrange("(a p) s -> p a s", p=128), w=[B_in[i]])
                    dma("sp", KT[i][:], rkT_d.ap()[:, tok].rearrange("(a p) s -> p a s", p=128), pw=[B_in[i]])
                    dma("sp", Vt[i][:], rv_d.ap()[tok, :], pw=[B_in[i]])
                    dma("sp", Gt[i][:], rg_d.ap()[tok, :], pw=[B_in[i]])

                def chunkA(c):
                    i = c % 2
                    pout, B_pout = pout2[i], B_pout2[i]
                    for a in range(4):
                        op("pe", lambda a=a: T.transpose(out=pkt_t[:, a, :], in_=KT[i][:, a, :], identity=identb[:]),
                           r=[B_in[i], B_const], pw=[B_pkt])
                    op("act", lambda: A.copy(out=Kt[i][:].rearrange("p (a s) -> p a s", a=4), in_=pkt),
                       r=[B_pkt], w=[B_Kt[i]])
                    op("dve", lambda: V.tensor_tensor(out=Vd[i][:], in0=Vt[i][:].rearrange("p (h e) -> p h e", h=NH),
                                                       in1=bc3(kdec, 64), op=ALU.mult),
                       r=[B_in[i], B_c3], w=[B_Vd[i]])
                    def scores(h):
                        a, hf = h // 2, h % 2
                        prt = slice(64 * hf, 64 * hf + 64)
                        si = cnt["sc"] % 2
                        cnt["sc"] += 1
                        ai = cnt["at"] % 3
                        cnt["at"] += 1
                        op("pe", lambda: T.matmul(psc[si], lhsT=KT[i][prt, a, :], rhs=QT[i][prt, a, :],
                                                  start=True, stop=True), r=[B_in[i]], w=[B_psc[si]])
                        op("dve", lambda: V.tensor_tensor(out=AT[ai][:], in0=psc[si], in1=DT[:, h, :], op=ALU.mult),
                           r=[B_psc[si], B_c3], w=[B_AT[ai]])
                        return ai

                    def inner(h, ai):
                        a, hf = h // 2, h % 2
                        prt = slice(64 * hf, 64 * hf + 64)
                        op("pe", lambda: T.matmul(pout[:, h, :], lhsT=AT[ai][:], rhs=Vt[i][:, h * 64:(h + 1) * 64],
                                                  start=True, stop=False),
                           r=[B_AT[ai], B_in[i]], pw=[B_pout], signal=False)
                        op("pe", lambda: T.matmul(pout[:, h, :], lhsT=QT[i][prt, a, :], rhs=stb[prt, a, :],
                                                  start=False, stop=True),
                           r=[B_in[i], B_stb], pw=[B_pout])

                    ais = [scores(0)]
                    for h in range(NH):
                        if h + 1 < NH:
                            ais.append(scores(h + 1))
                        inner(h, ais[h])
                    for h in range(NH):
                        a, hf = h // 2, h % 2
                        prt = slice(64 * hf, 64 * hf + 64)
                        op("pe", lambda: T.matmul(pstate_t[prt, a, :], lhsT=Kt[i][:, h * 64:(h + 1) * 64],
                                                  rhs=Vd[i][:, h, :], start=True, stop=True),
                           r=[B_Kt[i], B_Vd[i]], pw=[B_pstate], signal=(h == NH - 1))
                    op("dve", lambda: V.tensor_tensor(out=stf[:], in0=stf[:], in1=cdec[:], op=ALU.mult),
                       r=[B_c3], w=[B_stf])
                    op("dve", lambda: V.tensor_tensor(out=stf[:], in0=pstate, in1=stf[:], op=ALU.add),
                       r=[B_pstate], w=[B_stf])
                    op("act", lambda: A.copy(out=stb[:], in_=stf[:]), r=[B_stf], w=[B_stb])
                def chunkB(c):
                    i = c % 2
                    pout, B_pout = pout2[i], B_pout2[i]
                    op("dve", lambda: V.tensor_tensor(out=raw[:], in0=pout[:], in1=bc3(qdec, 64), op=ALU.mult),
                       r=[B_pout, B_c3], w=[B_raw])
                    op("dve", lambda: V.tensor_reduce(out=mean[:], in_=raw[:], op=ALU.add, axis=mybir.AxisListType.X),
                       r=[B_raw], w=[B_mean])
                    op("dve", lambda: V.tensor_scalar(out=mean[:], in0=mean[:], scalar1=1.0 / HD, scalar2=None,
                                                      op0=ALU.mult), r=[], w=[B_mean])
                    op("dve", lambda: V.tensor_tensor(out=cen[:], in0=raw[:], in1=bc3(mean, 64), op=ALU.subtract),
                       r=[B_raw, B_mean], w=[B_cen])
                    op("act", lambda: A.activation(out=sq[:], in_=cen[:], func=AF.Square),
                       r=[B_cen], w=[B_sq])
                    op("dve", lambda: V.tensor_reduce(out=var[:], in_=sq[:], op=ALU.add, axis=mybir.AxisListType.X),
                       r=[B_sq], w=[B_var])
                    op("act", lambda: A.activation(out=sdv[:], in_=var[:], func=AF.Sqrt, bias=epsg[:], scale=1.0 / HD),
                       r=[B_var, B_const], w=[B_sdv])
                    op("dve", lambda: V.reciprocal(out=rstd[:], in_=sdv[:]), r=[B_sdv], w=[B_rstd])
                    op("dve", lambda: V.tensor_tensor(out=nrm[:], in0=cen[:], in1=bc3(rstd, 64), op=ALU.mult),
                       r=[B_cen, B_rstd], w=[B_nrm])
                    op("act", lambda: A.activation(out=sg[:], in_=Gt[i][:], func=AF.Silu), r=[B_in[i]], w=[B_sg])
                    op("dve", lambda: V.tensor_tensor(out=res[i][:], in0=nrm[:].rearrange("p h e -> p (h e)"),
                                                       in1=sg[:], op=ALU.mult), r=[B_nrm, B_sg], w=[B_res[i]])
                    dma("pool", mixed_d.ap()[c * 128:(c + 1) * 128, 512:1024], res[i][:], r=[B_res[i]])

                load(0)
                if NB > 1:
                    load(1)
                chunkA(0)
                for c in range(NB):
                    if c + 1 < NB:
                        chunkA(c + 1)
                    chunkB(c)
                    if c + 2 < NB:
                        load(c + 2)
                sc.barrier(barr[:])

        def phase4a(l, hsrc):
            with contextlib.ExitStack() as st:
                wo = sbt(st, "wo", [128, 8, D], BF16)
                wu = sbt(st, "wu", [128, 8, 2 * DFF], BF16)
                B_wo, B_wu = Buf("wo"), Buf("wu")
                for k in range(8):
                    load_w(wo, k, w_out.ap()[l, k * 128:(k + 1) * 128, :], D, B_wo, 1024)
                for k in range(8):
                    load_w(wu, k, w_up.ap()[l, k * 128:(k + 1) * 128, :], 2 * DFF, B_wu, 1408)
                wn = sbt(st, "wn2", [128, D], F32)
                B_wn = Buf("wn")
                bcast_row(wn[:], ffn_norm_w, l * D, D, B_wn)
                nm = Normer(st, "n2", wn, B_wn)
                cwr = sbt(st, "cwr", [4 * NFC, 128], F32)
                cw = sbt(st, "cw", [128, 4 * NFC], F32)
                B_cwr, B_cw = Buf("cwr"), Buf("cw")
                for j in range(3):
                    dma("sp", cwr[j * NFC:(j + 1) * NFC, :],
                        bass.AP(conv_w, (l * 3 + j) * DFF, [[128, NFC], [1, 128]]), pw=[B_cwr])
                dma("sp", cwr[3 * NFC:4 * NFC, :], bass.AP(conv_b, l * DFF, [[128, NFC], [1, 128]]), pw=[B_cwr])
                pa = [pst(st, "pa%d" % i, [128, 512], F32) for i in range(2)]
                pg = [pst(st, "pg%d" % i, [128, 512], F32) for i in range(3)]
                B_pa = [Buf("pa") for _ in range(2)]
                B_pg = [Buf("pg") for _ in range(3)]
                po = [pst(st, "po4_%d" % i, [128, 512], F32) for i in range(1)]
                B_po = [Buf("po") for _ in range(1)]
                op("pe", lambda: T.transpose(out=po[0][:, 0:4 * NFC], in_=cwr[:], identity=identf[0:4 * NFC, 0:4 * NFC]),
                   r=[B_cwr, B_const], w=[B_po[0]])
                op("dve", lambda: V.tensor_copy(out=cw[:], in_=po[0][:, 0:4 * NFC]), r=[B_po[0]], w=[B_cw])
                hb = [sbt(st, "hb4_%d" % i, [128, D], F32) for i in range(4)]
                mb = [sbt(st, "mb%d" % i, [128, D], BF16) for i in range(4)]
                B_hb = [Buf("hb") for _ in range(4)]
                B_mb = [Buf("mb") for _ in range(4)]
                mT = [sbt(st, "mT%d" % i, [128, 8, 128], BF16) for i in range(2)]
                B_mT = [Buf("mT") for _ in range(2)]
                uT = [sbt(st, "uT4_%d" % i, [128, 8, 512], BF16) for i in range(2)]
                B_uT = [Buf("uT") for _ in range(2)]
                halo = sbt(st, "halo", [128, NFC, 2], F32)
                B_halo = [Buf("halo") for _ in range(NFC)]
                op("pool", lambda: P.memset(halo[:], 0.0), w=B_halo)
                ab = [sbt(st, "ab%d" % i, [128, 514], F32) for i in range(2)]
                yb = [sbt(st, "yb%d" % i, [128, 512], F32) for i in range(2)]
                gb = [sbt(st, "gb%d" % i, [128, 512], F32) for i in range(2)]
                ao = [sbt(st, "ao%d" % i, [128, 512], BF16) for i in range(3)]
                B_ab = [Buf("ab") for _ in range(2)]
                B_yb = [Buf("yb") for _ in range(2)]
                B_gb = [Buf("gb") for _ in range(2)]
                B_ao = [Buf("ao") for _ in range(3)]
                cnt = {"po": 0, "f": 0, "ao": 0}

                def pre_gen(t):
                    s = t % 2
                    for sub in range(4):
                        blk = t * 4 + sub
                        rows = slice(blk * 128, (blk + 1) * 128)
                        dma("sp", hb[sub][:], hsrc[rows, :], w=[B_hb[sub]])
                        dma("sp", mb[sub][:], mixed_d.ap()[rows, :], w=[B_mb[sub]])
                    ubs = {}

                    def stA(sub):
                        nm.transpose8(mb[sub], B_mb[sub], mT[sub % 2], B_mT[sub % 2], 0, 8)

                    def stB(sub):
                        blk = t * 4 + sub
                        i = sub
                        mi = sub % 2
                        rows = slice(blk * 128, (blk + 1) * 128)
                        for nh in range(2):
                            for k in range(8):
                                op("pe", lambda k=k: T.matmul(po[0][:], lhsT=mT[mi][:, k, :],
                                                              rhs=wo[:, k, nh * 512:(nh + 1) * 512],
                                                              start=(k == 0), stop=(k == 7)),
                                   r=[B_mT[mi], B_wo], pw=[B_po[0]], signal=(k == 7))
                            op("dve", lambda: V.tensor_tensor(out=hb[i][:, nh * 512:(nh + 1) * 512], in0=po[0][:],
                                                              in1=hb[i][:, nh * 512:(nh + 1) * 512], op=ALU.add),
                               r=[B_po[0]], w=[B_hb[i]])
                        dma("pool", hB.ap()[rows, :], hb[i][:], r=[B_hb[i]])
                        ubs[sub] = nm.run_a(hb[i][:], B_hb[i])

                    def stC(sub):
                        ub_, B_ub_ = ubs[sub]
                        nm.transpose8(ub_, B_ub_, uT[s], B_uT[s], sub * 128, 8)

                    order = [(stA, 0), (stB, 0), (stA, 1), (stC, 0), (stB, 1), (stA, 2), (stC, 1), (stB, 2),
                             (stA, 3), (stC, 2), (stB, 3), (stC, 3)]
                    for f, sub in order:
                        f(sub)
                        yield

                def pre_tile(t):
                    for _ in pre_gen(t):
                        pass

                def ffn_tile(t):
                    s = t % 2
                    tok = slice(t * 512, (t + 1) * 512)
                    gen = pre_gen(t + 1) if t + 1 < NT else None
                    for fc in range(NFC):
                        i = cnt["f"] % 2
                        gi3 = cnt["f"] % 3
                        cnt["f"] += 1
                        if gen is not None and (fc % 2 == 0 or fc == NFC - 1):
                            next(gen, None)
                        for k in range(8):
                            op("pe", lambda k=k: T.matmul(pa[i][:], lhsT=wu[:, k, fc * 128:(fc + 1) * 128],
                                                          rhs=uT[s][:, k, :], start=(k == 0), stop=(k == 7)),
                               r=[B_wu, B_uT[s]], pw=[B_pa[i]], signal=(k == 7))
                        for k in range(8):
                            op("pe", lambda k=k: T.matmul(pg[gi3][:], lhsT=wu[:, k, DFF + fc * 128:DFF + (fc + 1) * 128],
                                                          rhs=uT[s][:, k, :], start=(k == 0), stop=(k == 7)),
                               r=[B_wu, B_uT[s]], pw=[B_pg[gi3]], signal=(k == 7))
                        a_, y_, g_ = ab[i], yb[i], gb[i]
                        op("act", lambda: A.copy(out=a_[:, 0:2], in_=halo[:, fc, :]), r=[B_halo[fc]], w=[B_ab[i]])
                        op("act", lambda: A.copy(out=a_[:, 2:514], in_=pa[i][:]), r=[B_pa[i]], pw=[B_ab[i]])
                        op("act", lambda: A.copy(out=halo[:, fc, :], in_=a_[:, 512:514]),
                           r=[B_ab[i]], w=[B_halo[fc]])
                        op("dve", lambda: V.tensor_scalar(out=y_[:], in0=a_[:, 2:514],
                                                          scalar1=cw[:, 2 * NFC + fc:2 * NFC + fc + 1],
                                                          scalar2=cw[:, 3 * NFC + fc:3 * NFC + fc + 1],
                                                          op0=ALU.mult, op1=ALU.add),
                           r=[B_ab[i], B_cw], w=[B_yb[i]])
                        op("dve", lambda: V.scalar_tensor_tensor(out=y_[:], in0=a_[:, 1:513],
                                                                 scalar=cw[:, NFC + fc:NFC + fc + 1], in1=y_[:],
                                                                 op0=ALU.mult, op1=ALU.add),
                           r=[B_ab[i], B_cw], w=[B_yb[i]])
                        op("dve", lambda: V.scalar_tensor_tensor(out=y_[:], in0=a_[:, 0:512],
                                                                 scalar=cw[:, fc:fc + 1], in1=y_[:],
                                                                 op0=ALU.mult, op1=ALU.add),
                           r=[B_ab[i], B_cw], w=[B_yb[i]])
                        op("act", lambda: A.activation(out=g_[:], in_=y_[:], func=AF.Gelu), r=[B_yb[i]], w=[B_gb[i]])
                        oi = cnt["ao"] % 3
                        cnt["ao"] += 1
                        op("dve", lambda: V.tensor_tensor(out=ao[oi][:], in0=pg[gi3][:], in1=g_[:], op=ALU.mult),
                           r=[B_pg[gi3], B_gb[i]], w=[B_ao[oi]])
                        dma("pool", act_d.ap()[fc * 128:(fc + 1) * 128, tok], ao[oi][:], r=[B_ao[oi]])
                    if gen is not None:
                        for _ in gen:
                            pass

                pre_tile(0)
                for t in range(NT):
                    ffn_tile(t)
                sc.barrier(barr[:])

        def phase4b(l, last):
            with contextlib.ExitStack() as st:
                wd = sbt(st, "wd", [128, NFC, D], BF16)
                wg = sbt(st, "wg", [128, 8, D], BF16)
                wp = sbt(st, "wp", [128, 2, D], BF16)
                B_wd, B_wg, B_wp = Buf("wd"), Buf("wg"), Buf("wp")
                for k in range(NFC):
                    load_w(wd, k, w_down.ap()[l, k * 128:(k + 1) * 128, :], D, B_wd, 1024)
                for k in range(8):
                    load_w(wg, k, w_ple_gate.ap()[l, k * 128:(k + 1) * 128, :], D, B_wg, 1024)
                for k in range(2):
                    load_w(wp, k, w_ple_proj.ap()[l, k * 128:(k + 1) * 128, :], D, B_wp, 1024)
                wn = sbt(st, "wn3", [128, D], F32)
                B_wn = Buf("wn")
                bcast_row(wn[:], ple_norm_w, l * D, D, B_wn)
                nm = Normer(st, "n3", wn, B_wn)
                if last:
                    fw = sbt(st, "fw", [128, D], F32)
                    B_fw = Buf("fw")
                    bcast_row(fw[:], final_norm_w, 0, D, B_fw)
                    ot = [sbt(st, "ot%d" % i, [128, D], F32) for i in range(2)]
                    B_ot = [Buf("ot") for _ in range(2)]
                aT = [sbt(st, "aT%d" % i, [128, NFC, 512], BF16) for i in range(2)]
                B_aT = [Buf("aT") for _ in range(2)]
                hb = [sbt(st, "hb5_%d" % i, [128, D], F32) for i in range(3)]
                B_hb = [Buf("hb") for _ in range(3)]
                pf32 = [sbt(st, "pf32_%d" % i, [128, PLE], F32) for i in range(2)]
                pb = [sbt(st, "pb%d" % i, [128, PLE], BF16) for i in range(2)]
                B_pf32 = [Buf("pf32") for _ in range(2)]
                B_pb = [Buf("pb") for _ in range(2)]
                u3T = [sbt(st, "u3T%d" % i, [128, 8, 128], BF16) for i in range(2)]
                pT = [sbt(st, "pT%d" % i, [128, 2, 128], BF16) for i in range(2)]
                B_u3T = [Buf("u3T") for _ in range(2)]
                B_pT = [Buf("pT") for _ in range(2)]
                gate = [sbt(st, "gate%d" % i, [128, 512], F32) for i in range(2)]
                tmp = [sbt(st, "tmp%d" % i, [128, 512], F32) for i in range(2)]
                B_gate = [Buf("gate") for _ in range(2)]
                B_tmp = [Buf("tmp") for _ in range(2)]
                pd = [pst(st, "pd%d" % i, [128, 512], F32) for i in range(2)]
                B_pd = [Buf("pd") for _ in range(2)]
                pgt = [pst(st, "pgt%d" % i, [128, 512], F32) for i in range(2)]
                B_pgt = [Buf("pgt") for _ in range(2)]
                ppp = [pst(st, "ppp%d" % i, [128, 512], F32) for i in range(2)]
                B_ppp = [Buf("ppp") for _ in range(2)]
                cnt = {"pd": 0, "g": 0}

                def load_tile(t):
                    s = t % 2
                    src = act_d.ap()[:, t * 512:(t + 1) * 512].rearrange("(c p) s -> p c s", p=128)
                    dma("sp", aT[s][:], src, w=[B_aT[s]])

                def down(blk):
                    t, sub = blk // 4, blk % 4
                    s = t % 2
                    i = blk % 2
                    hi = blk % 3
                    rows = slice(blk * 128, (blk + 1) * 128)
                    dma("sp", hb[hi][:], hB.ap()[rows, :], w=[B_hb[hi]])
                    dma("sp", pf32[i][:], p_in.ap()[l, rows, :], w=[B_pf32[i]])
                    for nh in range(2):
                        pi = cnt["pd"] % 2
                        cnt["pd"] += 1
                        for fc in range(NFC):
                            op("pe", lambda fc=fc: T.matmul(pd[pi][:], lhsT=aT[s][:, fc, sub * 128:(sub + 1) * 128],
                                                            rhs=wd[:, fc, nh * 512:(nh + 1) * 512],
                                                            start=(fc == 0), stop=(fc == NFC - 1)),
                               r=[B_aT[s], B_wd], pw=[B_pd[pi]], signal=(fc == NFC - 1))
                        op("dve", lambda: V.tensor_tensor(out=hb[hi][:, nh * 512:(nh + 1) * 512], in0=pd[pi][:],
                                                          in1=hb[hi][:, nh * 512:(nh + 1) * 512], op=ALU.add),
                           r=[B_pd[pi]], w=[B_hb[hi]])
                    ub_, B_ub_ = nm.run_a(hb[hi][:], B_hb[hi])
                    op("act", lambda: A.copy(out=pb[i][:], in_=pf32[i][:]), r=[B_pf32[i]], w=[B_pb[i]])
                    return ub_, B_ub_

                def rest(blk, ub_, B_ub_):
                    i = blk % 2
                    hi = blk % 3
                    rows = slice(blk * 128, (blk + 1) * 128)
                    nm.transpose8(ub_, B_ub_, u3T[i], B_u3T[i], 0, 8)
                    nm.transpose8(pb[i], B_pb[i], pT[i], B_pT[i], 0, 2)
                    for nh in range(2):
                        gi = cnt["g"] % 2
                        cnt["g"] += 1
                        cs = slice(nh * 512, (nh + 1) * 512)
                        for k in range(8):
                            op("pe", lambda k=k: T.matmul(pgt[gi][:], lhsT=u3T[i][:, k, :], rhs=wg[:, k, cs],
                                                          start=(k == 0), stop=(k == 7)),
                               r=[B_u3T[i], B_wg], pw=[B_pgt[gi]], signal=(k == 7))
                        for k in range(2):
                            op("pe", lambda k=k: T.matmul(ppp[gi][:], lhsT=pT[i][:, k, :], rhs=wp[:, k, cs],
                                                          start=(k == 0), stop=(k == 1)),
                               r=[B_pT[i], B_wp], pw=[B_ppp[gi]], signal=(k == 1))
                        op("act", lambda: A.activation(out=gate[gi][:], in_=pgt[gi][:], func=AF.Sigmoid),
                           r=[B_pgt[gi]], w=[B_gate[gi]])
                        op("dve", lambda: V.tensor_tensor(out=tmp[gi][:], in0=ppp[gi][:], in1=gate[gi][:],
                                                          op=ALU.mult), r=[B_ppp[gi], B_gate[gi]], w=[B_tmp[gi]])
                        op("dve", lambda: V.tensor_tensor(out=hb[hi][:, cs], in0=hb[hi][:, cs], in1=tmp[gi][:],
                                                          op=ALU.add), r=[B_tmp[gi]], w=[B_hb[hi]])
                    if last:
                        rs, B_rs = nm.rstd(hb[hi][:], B_hb[hi], 2)
                        oi = blk % 2
                        op("dve", lambda: V.scalar_tensor_tensor(out=ot[oi][:], in0=hb[hi][:], scalar=rs[:],
                                                                 in1=fw[:], op0=ALU.mult, op1=ALU.mult),
                           r=[B_hb[hi], B_rs, B_fw], w=[B_ot[oi]])
                        dma("pool", out.ap()[rows, :], ot[oi][:], r=[B_ot[oi]])
                    else:
                        dma("pool", hA.ap()[rows, :], hb[hi][:], r=[B_hb[hi]])

                load_tile(0)
                if NT > 1:
                    load_tile(1)
                nxt = down(0)
                for blk in range(NB):
                    cur = nxt
                    if blk + 1 < NB:
                        if (blk + 1) % 4 == 0 and (blk + 1) // 4 + 1 < NT:
                            load_tile((blk + 1) // 4 + 1)
                        nxt = down(blk + 1)
                    rest(blk, *cur)
                sc.barrier(barr[:])

        for l in range(depth):
            hsrc = x.ap() if l == 0 else hA.ap()
            if "1" in phases:
                phase1(l, hsrc)
            if "2" in phases:
                phase2(l)
            if "3" in phases:
                phase3(l)
            if "4" in phases:
                phase4a(l, hsrc)
            if "5" in phases:
                phase4b(l, do_final and l == depth - 1)
        print("sched: ops=%d waits=%d" % (sc.nops, sc.nwait), sc.cnt)
    return nc


def make_consts(S):
    bf = ml_dtypes.bfloat16
    inv_freq = (10000.0 ** (-np.arange(0, HD, 2, dtype=np.float32) / HD)).astype(np.float32)
    ang = (np.arange(S, dtype=np.float32)[:, None] * inv_freq[None, :]).astype(np.float32)
    cos = np.cos(ang).astype(np.float32).T
    sin = np.sin(ang).astype(np.float32).T
    cos128 = np.concatenate([cos, cos, cos, cos], axis=0)
    sin128 = np.concatenate([-sin, sin, -sin, sin], axis=0)
    pswap = np.zeros((128, 128), np.float32)
    for d in range(128):
        base = (d // 64) * 64
        dd = d % 64
        pswap[base + (dd + 32) % 64, d] = 1.0
    s_idx = np.arange(128)[:, None]
    t_idx = np.arange(128)[None, :]
    negmask = np.where(s_idx > t_idx, NEG, 0.0).astype(np.float32)
    lg = np.log1p(-np.exp2(-5.0 - np.arange(NH, dtype=np.float64)))
    j = np.arange(128, dtype=np.float64)
    DT = np.zeros((128, NH, 128), np.float64)
    for h in range(NH):
        DT[:, h, :] = np.where(s_idx <= t_idx, np.exp(-lg[h] * (j[:, None] + 1.0)), 0.0)
    qdec = np.exp(lg[None, :] * (j[:, None] + 1.0))
    kdec = np.exp(lg[None, :] * (127.0 - j[:, None]))
    cdec = np.zeros((128, 4, 64), np.float64)
    for a in range(4):
        cdec[:64, a, :] = np.exp(lg[2 * a] * 128.0)
        cdec[64:, a, :] = np.exp(lg[2 * a + 1] * 128.0)
    return {
        "c_cosq": np.ascontiguousarray(cos128), "c_sinq": np.ascontiguousarray(sin128),
        "c_cosk": np.ascontiguousarray(cos128 * np.float32(0.125)),
        "c_sink": np.ascontiguousarray(sin128 * np.float32(0.125)),
        "c_identb": np.eye(128, dtype=np.float32).astype(bf), "c_identf": np.eye(128, dtype=np.float32),
        "c_pswap": pswap.astype(bf), "c_negmask": negmask.astype(bf),
        "c_DT": DT.reshape(128, NH * 128).astype(np.float32), "c_qdec": qdec.astype(np.float32),
        "c_kdec": kdec.astype(np.float32), "c_cdec": cdec.reshape(128, 256).astype(np.float32),
    }


_WNAMES = ["attn_norm_w", "w_in", "forget_bias", "w_out", "ffn_norm_w", "w_up", "conv_w", "conv_b", "w_down",
           "ple_norm_w", "w_ple_gate", "w_ple_proj", "final_norm_w"]


def kernel(**inputs):
    x = np.asarray(inputs["x"], dtype=np.float32)
    p = np.asarray(inputs["p"], dtype=np.float32)
    B, S, _ = x.shape
    depth = p.shape[0]
    nc = build(S, depth)
    consts = make_consts(S)
    shared = {k: np.ascontiguousarray(np.asarray(inputs[k], dtype=np.float32)) for k in _WNAMES}
    shared.update(consts)
    in_maps = []
    for b in range(B):
        m = dict(shared)
        m["x"] = np.ascontiguousarray(x[b])
        m["p"] = np.ascontiguousarray(p[:, b])
        in_maps.append(m)
    res = run_bass_kernel_spmd(nc, in_maps, core_ids=list(range(B)))
    return np.stack([np.asarray(r["out"], dtype=np.float32) for r in res.results], axis=0)
```

```python
import contextlib
import os
import numpy as np
import ml_dtypes
import concourse.bass as bass
import concourse.mybir as mybir
from concourse.bass_utils import run_bass_kernel_spmd

F32 = mybir.dt.float32
BF16 = mybir.dt.bfloat16
AF = mybir.ActivationFunctionType
ALU = mybir.AluOpType

D = 1024
HD = 64
NH = 8
DFF = 2816
NFC = DFF // 128
PLE = 256
INW = 3592
C_FQ, C_FK, C_FV, C_FL, C_RQ, C_RK, C_RV, C_RG = 0, 512, 1024, 1536, 1544, 2056, 2568, 3080
NORM_EPS = 1e-6
GN_EPS = 1e-5
NEG = -30000.0


class Buf:
    __slots__ = ("name", "w", "r")

    def __init__(self, name):
        self.name = name
        self.w = {}
        self.r = {}


def _key(tok):
    return tok[1] if tok[0] == "e" else (tok[1], tok[2])


def _put(d, tok):
    k = _key(tok)
    o = d.get(k)
    if o is None or o[-1] < tok[-1]:
        d[k] = tok


class Sched:
    def __init__(self, nc, stack):
        self.nc = nc
        self.eng = {"pe": nc.tensor, "act": nc.scalar, "dve": nc.vector, "pool": nc.gpsimd, "sp": nc.sync}
        self.sem = {k: stack.enter_context(nc.semaphore("s_" + k)) for k in self.eng}
        self.cnt = {k: 0 for k in self.eng}
        self.waited = {k: {} for k in self.eng}
        self.dq = {}
        for q, n in (("sp", 16), ("pool", 24), ("act", 2)):
            self.dq[q] = [[stack.enter_context(nc.semaphore("d_%s%d" % (q, i))), 0] for i in range(n)]
        self.dqi = {q: 0 for q in self.dq}
        self.nwait = 0
        self.nops = 0

    def _wait(self, e, tok):
        if tok[0] == "e":
            p, v = tok[1], tok[2]
            if p == e and e == "pe":
                return
            key = p
            sem = self.sem[p]
        else:
            q, i, v = tok[1], tok[2], tok[3]
            key = (q, i)
            sem = self.dq[q][i][0]
        if self.waited[e].get(key, 0) >= v:
            return
        self.waited[e][key] = v
        self.eng[e].wait_ge(sem, v)
        self.nwait += 1

    def _deps(self, e, r, w, pw):
        toks = []
        for b in r:
            toks += list(b.w.values())
        for b in w:
            toks += list(b.w.values())
            toks += list(b.r.values())
        for b in pw:
            toks += list(b.r.values())
        for t in toks:
            self._wait(e, t)

    def _upd(self, tok, r, w, pw):
        for b in r:
            _put(b.r, tok)
        for b in w:
            b.w = {_key(tok): tok}
            b.r = {}
        for b in pw:
            if b.r:
                b.w = {_key(tok): tok}
                b.r = {}
            else:
                _put(b.w, tok)

    def op(self, e, fn, r=(), w=(), pw=(), signal=True):
        self._deps(e, r, w, pw)
        inst = fn()
        self.nops += 1
        if signal:
            self.cnt[e] += 1
            inst.then_inc(self.sem[e], 1)
            tok = ("e", e, self.cnt[e])
        else:
            tok = ("e", e, self.cnt[e] + 1)
        self._upd(tok, r, w, pw)
        return tok

    def dma(self, q, out, in_, r=(), w=(), pw=(), **kw):
        i = self.dqi[q]
        self.dqi[q] = (i + 1) % len(self.dq[q])
        slot = self.dq[q][i]
        if slot[1] > 0:
            self._wait(q, ("d", q, i, slot[1]))
        self._deps(q, r, w, pw)
        inst = self.eng[q].dma_start(out=out, in_=in_, **kw)
        self.nops += 1
        slot[1] += 16
        inst.then_inc(slot[0], 16)
        tok = ("d", q, i, slot[1])
        self._upd(tok, r, w, pw)
        return tok

    def barrier(self, scratch):
        m = "dve"
        for p in self.eng:
            if p != m and self.cnt[p] > 0:
                self._wait(m, ("e", p, self.cnt[p]))
        for q in self.dq:
            for i, (s, c) in enumerate(self.dq[q]):
                if c > 0:
                    self._wait(m, ("d", q, i, c))
        if self.cnt[m] > 0:
            self._wait(m, ("e", m, self.cnt[m]))
        inst = self.nc.vector.memset(scratch, 0.0)
        self.cnt[m] += 1
        inst.then_inc(self.sem[m], 1)
        tok = ("e", m, self.cnt[m])
        for e in self.eng:
            if e != m:
                self._wait(e, tok)
            for p in self.eng:
                self.waited[e][p] = max(self.waited[e].get(p, 0), self.cnt[p] if p != m or e != m else self.cnt[p])
            for q in self.dq:
                for i, (s, c) in enumerate(self.dq[q]):
                    self.waited[e][(q, i)] = max(self.waited[e].get((q, i), 0), c)


def sb_ap(t, offset, pairs):
    return bass.AP(t, offset, [list(p) for p in pairs])


def build(S, depth, do_final=True, phases="12345"):
    NB = S // 128
    NT = S // 512
    nc = bass.Bass("TRN2", target_bir_lowering=False)

    def din(name, shape, dt=F32):
        return nc.dram_tensor(name, list(shape), dt, kind="ExternalInput")

    def dscr(name, shape, dt):
        return nc.dram_tensor(name, list(shape), dt, kind="Internal")

    x = din("x", [S, D])
    p_in = din("p", [depth, S, PLE])
    attn_norm_w = din("attn_norm_w", [depth, D])
    w_in = din("w_in", [depth, D, INW])
    forget_bias = din("forget_bias", [depth, NH])
    w_out = din("w_out", [depth, D, D])
    ffn_norm_w = din("ffn_norm_w", [depth, D])
    w_up = din("w_up", [depth, D, 2 * DFF])
    conv_w = din("conv_w", [depth, 3, DFF])
    conv_b = din("conv_b", [depth, DFF])
    w_down = din("w_down", [depth, DFF, D])
    ple_norm_w = din("ple_norm_w", [depth, D])
    w_ple_gate = din("w_ple_gate", [depth, D, D])
    w_ple_proj = din("w_ple_proj", [depth, PLE, D])
    final_norm_w = din("final_norm_w", [D])
    c_cosq = din("c_cosq", [128, S])
    c_sinq = din("c_sinq", [128, S])
    c_cosk = din("c_cosk", [128, S])
    c_sink = din("c_sink", [128, S])
    c_identb = din("c_identb", [128, 128], BF16)
    c_identf = din("c_identf", [128, 128])
    c_pswap = din("c_pswap", [128, 128], BF16)
    c_negmask = din("c_negmask", [128, 128], BF16)
    c_DT = din("c_DT", [128, NH * 128])
    c_qdec = din("c_qdec", [128, NH])
    c_kdec = din("c_kdec", [128, NH])
    c_cdec = din("c_cdec", [128, 4 * 64])

    out = nc.dram_tensor("out", [S, D], F32, kind="ExternalOutput")

    hA = dscr("hA", [S, D], F32)
    hB = dscr("hB", [S, D], F32)
    fqT_d = dscr("fqT_d", [NH, 65, S], BF16)
    fkT_d = dscr("fkT_d", [NH, 64, S], BF16)
    fv_d = dscr("fv_d", [S, 512], BF16)
    rqT_d = dscr("rqT_d", [512, S], BF16)
    rkT_d = dscr("rkT_d", [512, S], BF16)
    rv_d = dscr("rv_d", [S, 512], BF16)
    rg_d = dscr("rg_d", [S, 512], BF16)
    mixed_d = dscr("mixed_d", [S, 1024], BF16)
    act_d = dscr("act_d", [DFF, S], BF16)

    with contextlib.ExitStack() as gst:
        G = gst.enter_context
        sc = Sched(nc, gst)
        op, dma = sc.op, sc.dma
        V, A, P, T = nc.vector, nc.scalar, nc.gpsimd, nc.tensor

        uid = {"n": 0}

        def sbt(st, name, shape, dt):
            uid["n"] += 1
            return st.enter_context(nc.sbuf_tensor("%s_%d" % (name, uid["n"]), list(shape), dt))

        def pst(st, name, shape, dt):
            uid["n"] += 1
            return st.enter_context(nc.psum_tensor("%s_%d" % (name, uid["n"]), list(shape), dt))

        identb = sbt(gst, "identb", [128, 128], BF16)
        identf = sbt(gst, "identf", [128, 128], F32)
        pswap = sbt(gst, "pswap", [128, 128], BF16)
        negmask = sbt(gst, "negmask", [128, 128], BF16)
        epsn = sbt(gst, "epsn", [128, 1], F32)
        epsg = sbt(gst, "epsg", [128, 1], F32)
        barr = sbt(gst, "barr", [128, 1], F32)
        negcum = sbt(gst, "negcum", [128, NB, NH], F32)
        B_const = Buf("const")
        B_negcum = Buf("negcum")
        dma("sp", identb[:], c_identb.ap(), w=[B_const])
        dma("sp", identf[:], c_identf.ap(), pw=[B_const])
        dma("sp", pswap[:], c_pswap.ap(), pw=[B_const])
        dma("sp", negmask[:], c_negmask.ap(), pw=[B_const])
        op("pool", lambda: P.memset(epsn[:], NORM_EPS), pw=[B_const])
        op("pool", lambda: P.memset(epsg[:], GN_EPS), pw=[B_const])
        sc.barrier(barr[:])

        rr = {"i": 0}

        def evac_engine():
            rr["i"] += 1
            return "act" if rr["i"] % 2 else "dve"

        def copy_on(e, out_ap, in_ap, r, w=(), pw=(), scale=None):
            if e == "act":
                if scale is None:
                    return op("act", lambda: A.copy(out=out_ap, in_=in_ap), r=r, w=w, pw=pw)
                return op("act", lambda: A.mul(out=out_ap, in_=in_ap, mul=scale), r=r, w=w, pw=pw)
            if e == "dve":
                if scale is None:
                    return op("dve", lambda: V.tensor_copy(out=out_ap, in_=in_ap), r=r, w=w, pw=pw)
                return op("dve", lambda: V.tensor_scalar(out=out_ap, in0=in_ap, scalar1=scale, scalar2=None,
                                                         op0=ALU.mult), r=r, w=w, pw=pw)
            if scale is None:
                return op("pool", lambda: P.tensor_copy(out=out_ap, in_=in_ap), r=r, w=w, pw=pw)
            return op("pool", lambda: P.tensor_scalar(out=out_ap, in0=in_ap, scalar1=scale, scalar2=None,
                                                      op0=ALU.mult), r=r, w=w, pw=pw)

        def load_w(dst, k, src_rows_ap, ncols, B, piece):
            c0 = 0
            while c0 < ncols:
                c1 = min(ncols, c0 + piece)
                dma("pool", dst[:, k, c0:c1], src_rows_ap[:, c0:c1], pw=[B])
                c0 = c1

        def bcast_row(dst, src_t, off, n, B):
            src = bass.AP(src_t, off, [[0, 128], [1, n]])
            dma("pool", dst, src, w=[B])

        class Normer:
            def __init__(self, st, tag, wn, B_wn):
                self.wn, self.B_wn = wn, B_wn
                self.junk = sbt(st, tag + "junk", [128, D], BF16)
                self.B_junk = Buf("junk")
                self.ss = [sbt(st, tag + "ss%d" % i, [128, 1], F32) for i in range(3)]
                self.sd = [sbt(st, tag + "sd%d" % i, [128, 1], F32) for i in range(3)]
                self.rs = [sbt(st, tag + "rs%d" % i, [128, 1], F32) for i in range(3)]
                self.B_ss = [Buf("ss") for _ in range(3)]
                self.B_sd = [Buf("sd") for _ in range(3)]
                self.B_rs = [Buf("rs") for _ in range(3)]
                self.ub = [sbt(st, tag + "ub%d" % i, [128, D], BF16) for i in range(2)]
                self.B_ub = [Buf("ub") for _ in range(2)]
                self.ptr = [pst(st, tag + "ptr%d" % i, [128, 8, 128], BF16) for i in range(2)]
                self.B_ptr = [Buf("ptr0"), Buf("ptr1")]
                self.i = 0

            def rstd(self, hb, B_hb, i=None):
                if i is None:
                    i = self.i % 2
                ss, sd, rs = self.ss[i], self.sd[i], self.rs[i]
                op("act", lambda: A.activation(out=self.junk[:], in_=hb, func=AF.Square, accum_out=ss[:]),
                   r=[B_hb], w=[self.B_junk, self.B_ss[i]])
                op("act", lambda: A.activation(out=sd[:], in_=ss[:], func=AF.Sqrt, bias=epsn[:], scale=1.0 / D),
                   r=[self.B_ss[i], B_const], w=[self.B_sd[i]])
                op("dve", lambda: V.reciprocal(out=rs[:], in_=sd[:]), r=[self.B_sd[i]], w=[self.B_rs[i]])
                return rs, self.B_rs[i]

            def run_a(self, hb, B_hb):
                i = self.i % 2
                rs, B_rs = self.rstd(hb, B_hb)
                ub = self.ub[i]
                op("dve", lambda: V.scalar_tensor_tensor(out=ub[:], in0=hb, scalar=rs[:], in1=self.wn[:],
                                                         op0=ALU.mult, op1=ALU.mult),
                   r=[B_hb, B_rs, self.B_wn], w=[self.B_ub[i]])
                self.i += 1
                return ub, self.B_ub[i]

            def run(self, hb, B_hb, uT, B_uT, c0):
                ub, B_ub = self.run_a(hb, B_hb)
                self.transpose8(ub, B_ub, uT, B_uT, c0, 8)

            def transpose8(self, ub, B_ub, uT, B_uT, c0, nk):
                for j in range((nk + 3) // 4):
                    n = min(4, nk - 4 * j)
                    for kk in range(n):
                        k = 4 * j + kk
                        op("pe", lambda k=k, kk=kk, j=j: T.transpose(out=self.ptr[j][:, kk, :],
                                                                     in_=ub[:, k * 128:(k + 1) * 128],
                                                                     identity=identb[:]),
                           r=[B_ub, B_const], pw=[self.B_ptr[j]])
                    copy_on(evac_engine(), uT[:, 4 * j:4 * j + n, c0:c0 + 128], self.ptr[j][:, 0:n, :],
                            r=[self.B_ptr[j]], pw=[B_uT])

        def phase1(l, hsrc):
            with contextlib.ExitStack() as st:
                win = sbt(st, "win", [128, 8, INW], BF16)
                B_win = Buf("win")
                for k in range(8):
                    load_w(win, k, w_in.ap()[l, k * 128:(k + 1) * 128, :], INW, B_win, 1796)
                wn = sbt(st, "wn1", [128, D], F32)
                B_wn = Buf("wn")
                bcast_row(wn[:], attn_norm_w, l * D, D, B_wn)
                fb = sbt(st, "fb", [NH, 1], F32)
                nfb = sbt(st, "nfb", [NH, 1], F32)
                B_fb, B_nfb = Buf("fb"), Buf("nfb")
                dma("sp", fb[:], bass.AP(forget_bias, l * NH, [[1, NH], [1, 1]]), w=[B_fb])
                op("dve", lambda: V.tensor_scalar(out=nfb[:], in0=fb[:], scalar1=-1.0, scalar2=None, op0=ALU.mult),
                   r=[B_fb], w=[B_nfb])
                nm = Normer(st, "n1", wn, B_wn)
                hb = [sbt(st, "hb%d" % i, [128, D], F32) for i in range(4)]
                B_hb = [Buf("hb") for _ in range(4)]
                uT = [sbt(st, "uT%d" % i, [128, 8, 512], BF16) for i in range(2)]
                B_uT = [Buf("uT") for _ in range(2)]
                tabs = [[sbt(st, "tab%d_%d" % (i, j), [128, 512], F32) for j in range(4)] for i in range(2)]
                B_tabs = [Buf("tabs") for _ in range(2)]
                pf = [pst(st, "pf%d" % i, [128, 512], F32) for i in range(2)]
                B_pf = [Buf("pf") for _ in range(2)]
                pt = [pst(st, "pt%d" % i, [128, 512], F32) for i in range(2)]
                B_pt = [Buf("pt") for _ in range(2)]
                pr = pst(st, "pr", [128, 512], F32)
                B_pr = Buf("pr")
                plnt = pst(st, "plnt", [128, 512], F32)
                pl = plnt[0:NH, :]
                pnt = plnt[:, 0:4 * NH].rearrange("p (j h) -> p j h", j=4)
                B_pl = Buf("plnt")
                B_pnt = B_pl
                NST = 4
                stg = [sbt(st, "stg%d" % i, [128, 512], BF16) for i in range(NST)]
                B_stg = [Buf("stg") for _ in range(NST)]
                qb = [sbt(st, "qb%d" % i, [128, 512], BF16) for i in range(2)]
                B_qb = [Buf("qb") for _ in range(2)]
                t1 = [sbt(st, "t1_%d" % i, [128, 512], F32) for i in range(2)]
                t2 = [sbt(st, "t2_%d" % i, [128, 512], F32) for i in range(2)]
                B_t1 = [Buf("t1") for _ in range(2)]
                B_t2 = [Buf("t2") for _ in range(2)]
                ef = sbt(st, "ef", [NH, 512], F32)
                lf = sbt(st, "lf", [NH, 512], F32)
                B_ef, B_lf = Buf("ef"), Buf("lf")
                ones8 = sbt(st, "ones8", [NH, 512], F32)
                B_ones8 = Buf("ones8")
                op("pool", lambda: P.memset(ones8[:], 1.0), w=[B_ones8])
                ncT = [sbt(st, "ncT%d" % i, [NH, 512], F32) for i in range(2)]
                B_ncT = [Buf("ncT") for _ in range(2)]
                cb = sbt(st, "cb", [NH, 512], BF16)
                B_cb = Buf("cb")
                cnt = {"pf": 0, "pt": 0, "stg": 0, "rot": 0}

                def norm_gen(t):
                    s = t % 2
                    tb = tabs[s]
                    for j, c in enumerate((c_cosq, c_sinq, c_cosk, c_sink)):
                        dma("sp", tb[j][:], c.ap()[:, t * 512:(t + 1) * 512], pw=[B_tabs[s]])
                    for sub in range(4):
                        blk = t * 4 + sub
                        dma("sp", hb[sub][:], hsrc[blk * 128:(blk + 1) * 128, :], w=[B_hb[sub]])
                    for sub in range(4):
                        i = sub
                        ub_, B_ub_ = nm.run_a(hb[i][:], B_hb[i])
                        yield
                        nm.transpose8(ub_, B_ub_, uT[s], B_uT[s], sub * 128, 8)
                        yield

                def norm_tile(t):
                    for _ in norm_gen(t):
                        pass

                def fmajor(t, c0, M=128):
                    s = t % 2
                    i = cnt["pf"] % 2
                    cnt["pf"] += 1
                    for k in range(8):
                        op("pe", lambda k=k: T.matmul(pf[i][0:M, :], lhsT=win[:, k, c0:c0 + M], rhs=uT[s][:, k, :],
                                                      start=(k == 0), stop=(k == 7)),
                           r=[B_win, B_uT[s]], pw=[B_pf[i]], signal=(k == 7))
                    return pf[i], B_pf[i]

                def next_stg():
                    i = cnt["stg"] % NST
                    cnt["stg"] += 1
                    return stg[i], B_stg[i]

                def mm_tile(t, lvl=9):
                    s = t % 2
                    tok = slice(t * 512, (t + 1) * 512)
                    gen = norm_gen(t + 1) if t + 1 < NT else None
                    pc = {"n": 0}

                    def pull():
                        pc["n"] += 1
                        if gen is not None and pc["n"] % 3 == 0:
                            next(gen, None)
                    for grp, c0, dst, scale in (("fq", C_FQ, fqT_d, 0.125), ("fk", C_FK, fkT_d, None)):
                        for cc in range(4):
                            ps, B_ps = fmajor(t, c0 + cc * 128)
                            sg, B_sg = next_stg()
                            copy_on(evac_engine(), sg[:], ps[:], r=[B_ps], w=[B_sg], scale=scale)
                            for hh in range(2):
                                dma("pool", dst.ap()[2 * cc + hh, 0:64, tok], sg[hh * 64:(hh + 1) * 64, :], r=[B_sg])
                            pull()
                    if lvl < 3:
                        return
                    i = cnt["pf"] % 2
                    cnt["pf"] += 1
                    for k in range(8):
                        op("pe", lambda k=k: T.matmul(pl, lhsT=win[:, k, C_FL:C_FL + NH], rhs=uT[s][:, k, :],
                                                      start=(k == 0), stop=(k == 7)),
                           r=[B_win, B_uT[s]], w=([B_pl] if k == 0 else []), pw=([] if k == 0 else [B_pl]),
                           signal=(k == 7))
                    op("act", lambda: A.activation(out=ef[:], in_=pl, func=AF.Exp, bias=nfb[:], scale=-1.0),
                       r=[B_pl, B_nfb], w=[B_ef])
                    op("act", lambda: A.activation(out=lf[:], in_=ef[:], func=AF.Ln, bias=1.0, scale=1.0),
                       r=[B_ef], w=[B_lf])
                    ci = t % 2
                    init = 0.0 if t == 0 else ncT[1 - ci][:, 511:512]
                    op("dve", lambda: V.tensor_tensor_scan(out=ncT[ci][:], data0=ones8[:], data1=lf[:], initial=init,
                                                           op0=ALU.mult, op1=ALU.add),
                       r=[B_lf, B_ones8, B_ncT[1 - ci]], w=[B_ncT[ci]])
                    op("dve", lambda: V.tensor_scalar(out=cb[:], in0=ncT[ci][:], scalar1=-1.0, scalar2=None,
                                                      op0=ALU.mult), r=[B_ncT[ci]], w=[B_cb])
                    dma("pool", fqT_d.ap()[:, 64, tok], cb[:], r=[B_cb])
                    for j in range(4):
                        op("pe", lambda j=j: T.transpose(out=pnt[:, j, :], in_=ncT[ci][:, j * 128:(j + 1) * 128],
                                                         identity=identf[0:NH, 0:NH]),
                           r=[B_ncT[ci], B_const], w=([B_pnt] if j == 0 else []), pw=([] if j == 0 else [B_pnt]))
                    op("dve", lambda: V.tensor_copy(out=negcum[:, t * 4:(t + 1) * 4, :], in_=pnt),
                       r=[B_pnt], pw=[B_negcum])
                    if lvl < 4:
                        return
                    deferred = {"f": None}
                    for grp, c0, dst, tj in (("rq", C_RQ, rqT_d, 0), ("rk", C_RK, rkT_d, 2)):
                        for cc in range(4):
                            ps, B_ps = fmajor(t, c0 + cc * 128)
                            ri = cnt["rot"] % 2
                            cnt["rot"] += 1
                            op("act", lambda: A.copy(out=qb[ri][:], in_=ps[:]), r=[B_ps], w=[B_qb[ri]])
                            op("dve", lambda: V.tensor_tensor(out=t1[ri][:], in0=ps[:], in1=tabs[s][tj][:],
                                                              op=ALU.mult), r=[B_ps, B_tabs[s], B_qb[ri]], w=[B_t1[ri]])
                            if deferred["f"] is not None:
                                deferred["f"]()

                            def rot_rest(ri=ri, tj=tj, dst=dst, cc=cc):
                                op("pe", lambda: T.matmul(pr[:], lhsT=pswap[:], rhs=qb[ri][:], start=True, stop=True),
                                   r=[B_qb[ri], B_const], w=[B_pr])
                                op("dve", lambda: V.tensor_tensor(out=t2[ri][:], in0=pr[:], in1=tabs[s][tj + 1][:],
                                                                  op=ALU.mult), r=[B_pr, B_tabs[s]], w=[B_t2[ri]])
                                sg, B_sg = next_stg()
                                op("dve", lambda: V.tensor_tensor(out=sg[:], in0=t1[ri][:], in1=t2[ri][:], op=ALU.add),
                                   r=[B_t1[ri], B_t2[ri]], w=[B_sg])
                                dma("pool", dst.ap()[cc * 128:(cc + 1) * 128, tok], sg[:], r=[B_sg])
                                deferred["f"] = None
                            deferred["f"] = rot_rest
                            pull()
                    if lvl < 5:
                        return
                    for sub in range(4):
                        rows = slice(t * 512 + sub * 128, t * 512 + (sub + 1) * 128)
                        for c0, dst in ((C_FV, fv_d), (C_RV, rv_d), (C_RG, rg_d)):
                            i = cnt["pt"] % 2
                            cnt["pt"] += 1
                            for k in range(8):
                                op("pe", lambda k=k: T.matmul(pt[i][:], lhsT=uT[s][:, k, sub * 128:(sub + 1) * 128],
                                                              rhs=win[:, k, c0:c0 + 512], start=(k == 0), stop=(k == 7)),
                                   r=[B_win, B_uT[s]], pw=[B_pt[i]], signal=(k == 7))
                            if deferred["f"] is not None:
                                deferred["f"]()
                            sg, B_sg = next_stg()
                            copy_on(evac_engine(), sg[:], pt[i][:], r=[B_pt[i]], w=[B_sg])
                            dma("pool", dst.ap()[rows, :], sg[:], r=[B_sg])
                            pull()
                    if gen is not None:
                        for _ in gen:
                            pass

                norm_tile(0)
                for t in range(NT):
                    mm_tile(t)
                sc.barrier(barr[:])

        def phase2(l):
            with contextlib.ExitStack() as st:
                KT = [sbt(st, "KT%d" % i, [65, S], BF16) for i in range(2)]
                QT = [sbt(st, "QT%d" % i, [65, S], BF16) for i in range(2)]
                VA = [sbt(st, "VA%d" % i, [128, NB, 65], BF16) for i in range(2)]
                B_KT = [Buf("KT") for _ in range(2)]
                B_QT = [Buf("QT") for _ in range(2)]
                B_VA = [Buf("VA") for _ in range(2)]
                for i in range(2):
                    op("pool", lambda i=i: P.memset(KT[i][64:65, :], 1.0), pw=[B_KT[i]])
                    op("pool", lambda i=i: P.memset(VA[i][:, :, 64:65], 1.0), pw=[B_VA[i]])
                NPS = 3
                ps = [pst(st, "ps%d" % i, [128, 512], F32) for i in range(NPS)]
                B_ps = [Buf("ps") for _ in range(NPS)]
                poT = [pst(st, "poT%d" % i, [128, 512], F32) for i in range(2)]
                B_poT = [Buf("poT") for _ in range(2)]
                pfin = pst(st, "pfin", [128, 4, 128], F32)
                B_pfin = Buf("pfin")
                NPT = 4
                PT = [sbt(st, "PT%d" % i, [128, 512], BF16) for i in range(NPT)]
                B_PT = [Buf("PT") for _ in range(NPT)]
                oT = [sbt(st, "oT%d" % i, [65, 512], F32) for i in range(2)]
                B_oT = [Buf("oT") for _ in range(2)]
                rsum = [sbt(st, "rsum%d" % i, [128, 4], F32) for i in range(2)]
                B_rsum = [Buf("rsum") for _ in range(2)]
                fo = [sbt(st, "fo%d" % i, [128, 4, 64], BF16) for i in range(2)]
                B_fo = [Buf("fo") for _ in range(2)]
                cnt = {"ps": 0, "fo": 0}

                def load_head(h):
                    i = h % 2
                    dma("sp", KT[i][0:64, :], fkT_d.ap()[h], pw=[B_KT[i]], r=[])
                    dma("sp", QT[i][0:65, :], fqT_d.ap()[h], w=[B_QT[i]])
                    src = fv_d.ap()[:, h * 64:(h + 1) * 64].rearrange("(n p) e -> p n e", p=128)
                    dma("sp", VA[i][:, :, 0:64], src, pw=[B_VA[i]])

                def emit_qk(stp):
                    h, qt, kb = stp
                    i = h % 2
                    q0 = qt * 512
                    di = kb - 4 * qt
                    j0 = max(di, 0)
                    N = 512 - 128 * j0
                    pi = cnt["ps"] % NPS
                    cnt["ps"] += 1
                    diag = di >= 0
                    op("pe", lambda: T.matmul(ps[pi][:, 0:N], lhsT=KT[i][0:65, kb * 128:(kb + 1) * 128],
                                              rhs=QT[i][0:65, q0 + 128 * j0:q0 + 512], start=True, stop=not diag),
                       r=[B_KT[i], B_QT[i]], w=[B_ps[pi]], signal=not diag)
                    if diag:
                        op("pe", lambda: T.matmul(ps[pi][:, 0:128], lhsT=identb[:], rhs=negmask[:],
                                                  start=False, stop=True), r=[B_const], pw=[B_ps[pi]])
                    return pi

                def emit_rest(stp, pi, fi):
                    h, qt, kb = stp
                    i = h % 2
                    nkb = 4 * qt + 4
                    j0 = max(kb - 4 * qt, 0)
                    N = 512 - 128 * j0
                    ti = cnt["pt"] % NPT
                    cnt["pt"] += 1
                    op("act", lambda: A.activation(out=PT[ti][:, 0:N], in_=ps[pi][:, 0:N], func=AF.Exp,
                                                   bias=negcum[:, kb, h:h + 1], scale=1.0),
                       r=[B_ps[pi], B_negcum], w=[B_PT[ti]])
                    op("pe", lambda: T.matmul(poT[fi][0:65, 128 * j0:512], lhsT=VA[i][:, kb, :],
                                              rhs=PT[ti][:, 0:N], start=(kb == 0), stop=(kb == nkb - 1)),
                       r=[B_PT[ti], B_VA[i]], pw=[B_poT[fi]])

                def fin_a(fi):
                    op("dve", lambda: V.tensor_copy(out=oT[fi][:], in_=poT[fi][0:65, :]), r=[B_poT[fi]], w=[B_oT[fi]])

                def fin_b(h, qt, fi):
                    q0 = qt * 512
                    for j in range(4):
                        op("pe", lambda j=j: T.transpose(out=pfin[:, j, 0:65], in_=oT[fi][0:65, j * 128:(j + 1) * 128],
                                                         identity=identf[0:65, 0:65]),
                           r=[B_oT[fi], B_const], w=([B_pfin] if j == 0 else []), pw=([] if j == 0 else [B_pfin]))
                    op("dve", lambda: V.reciprocal(out=rsum[fi][:].rearrange("p (j o) -> p j o", o=1),
                                                   in_=pfin[:, :, 64:65]), r=[B_pfin], w=[B_rsum[fi]])
                    op("dve", lambda: V.tensor_tensor(out=fo[fi][:], in0=pfin[:, :, 0:64],
                                                      in1=sb_ap(rsum[fi], 0, [[4, 128], [1, 4], [0, 64]]),
                                                      op=ALU.mult),
                       r=[B_pfin, B_rsum[fi]], w=[B_fo[fi]])
                    dst = mixed_d.ap()[q0:q0 + 512, h * 64:(h + 1) * 64].rearrange("(j p) e -> p j e", p=128)
                    dma("pool", dst, fo[fi][:], r=[B_fo[fi]])

                cnt["pt"] = 0
                steps = [(h, qt, kb) for h in range(NH) for qt in range(NT) for kb in range(4 * qt + 4)]
                LOOK = 2
                load_head(0)
                pis = [emit_qk(steps[n]) for n in range(min(LOOK, len(steps)))]
                pending = []
                tile_no = 0
                for n, stp in enumerate(steps):
                    h, qt, kb = stp
                    if qt == 0 and kb == 0 and h + 1 < NH:
                        load_head(h + 1)
                    if n + LOOK < len(steps):
                        pis.append(emit_qk(steps[n + LOOK]))
                    fi = tile_no % 2
                    emit_rest(stp, pis[n], fi)
                    for pnd in pending:
                        pnd[0] -= 1
                    while pending and pending[0][0] <= 0:
                        _, ph, pq, pf_ = pending.pop(0)
                        fin_b(ph, pq, pf_)
                    if kb == 4 * qt + 3:
                        fin_a(fi)
                        pending.append([2, h, qt, fi])
                        tile_no += 1
                for _, ph, pq, pf_ in pending:
                    fin_b(ph, pq, pf_)
                sc.barrier(barr[:])

        def phase3(l):
            with contextlib.ExitStack() as st:
                DT = sbt(st, "DT", [128, NH, 128], F32)
                qdec = sbt(st, "qdec", [128, NH], F32)
                kdec = sbt(st, "kdec", [128, NH], F32)
                cdec = sbt(st, "cdec", [128, 4, 64], F32)
                B_c3 = Buf("c3")
                dma("sp", DT[:], c_DT.ap().rearrange("p (h i) -> p h i", h=NH), w=[B_c3])
                dma("sp", qdec[:], c_qdec.ap(), pw=[B_c3])
                dma("sp", kdec[:], c_kdec.ap(), pw=[B_c3])
                dma("sp", cdec[:], c_cdec.ap().rearrange("p (a e) -> p a e", a=4), pw=[B_c3])
                stf = sbt(st, "stf", [128, 4, 64], F32)
                stb = sbt(st, "stb", [128, 4, 64], BF16)
                B_stf, B_stb = Buf("stf"), Buf("stb")
                op("pool", lambda: P.memset(stf[:], 0.0), w=[B_stf])
                op("pool", lambda: P.memset(stb[:], 0.0), w=[B_stb])
                QT = [sbt(st, "rQT%d" % i, [128, 4, 128], BF16) for i in range(3)]
                KT = [sbt(st, "rKT%d" % i, [128, 4, 128], BF16) for i in range(3)]
                Vt = [sbt(st, "rV%d" % i, [128, 512], BF16) for i in range(3)]
                Gt = [sbt(st, "rG%d" % i, [128, 512], BF16) for i in range(3)]
                B_in = [Buf("r_in") for _ in range(3)]
                Kt = [sbt(st, "rKt%d" % i, [128, 512], BF16) for i in range(2)]
                B_Kt = [Buf("Kt") for _ in range(2)]
                Vd = [sbt(st, "rVd%d" % i, [128, NH, 64], BF16) for i in range(2)]
                B_Vd = [Buf("Vd") for _ in range(2)]
                AT = [sbt(st, "rAT%d" % i, [128, 128], BF16) for i in range(3)]
                B_AT = [Buf("AT") for _ in range(3)]
                pkt_t = pst(st, "pkt", [128, 8, 128], BF16)
                pkt = pkt_t[:, 0:4, :]
                B_pkt = Buf("pkt")
                psc_t = [pst(st, "psc%d" % i, [128, 512], F32) for i in range(2)]
                psc = [t_[:, 0:128] for t_ in psc_t]
                B_psc = [Buf("psc") for _ in range(2)]
                pout2 = [pst(st, "pout%d" % i, [128, NH, 64], F32) for i in range(2)]
                B_pout2 = [Buf("pout") for _ in range(2)]
                pstate_t = pst(st, "pstate", [128, 8, 64], F32)
                pstate = pstate_t[:, 0:4, :]
                B_pstate = Buf("pstate")
                raw = sbt(st, "raw", [128, NH, 64], F32)
                cen = sbt(st, "cen", [128, NH, 64], F32)
                sq = sbt(st, "sq", [128, NH, 64], F32)
                nrm = sbt(st, "nrm", [128, NH, 64], F32)
                sg = sbt(st, "sg", [128, 512], F32)
                B_raw, B_cen, B_sq, B_nrm, B_sg = Buf("raw"), Buf("cen"), Buf("sq"), Buf("nrm"), Buf("sg")
                mean = sbt(st, "mean", [128, NH], F32)
                var = sbt(st, "var", [128, NH], F32)
                sdv = sbt(st, "sdv", [128, NH], F32)
                rstd = sbt(st, "rstd", [128, NH], F32)
                B_mean, B_var, B_sdv, B_rstd = Buf("mean"), Buf("var"), Buf("sdv"), Buf("rstd")
                res = [sbt(st, "res%d" % i, [128, 512], BF16) for i in range(2)]
                B_res = [Buf("res") for _ in range(2)]
                cnt = {"sc": 0, "at": 0}

                def bc3(t, n):
                    return sb_ap(t, 0, [[NH, 128], [1, NH], [0, n]])

                def load(c):
                    i = c % 3
                    tok = slice(c * 128, (c + 1) * 128)
                    dma("sp", QT[i][:], rqT_d.ap()[:, tok].rearrange("(a p) s -> p a s", p=128), w=[B_in[i]])
                    dma("sp", KT[i][:], rkT_d.ap()[:, tok].rearrange("(a p) s -> p a s", p=128), pw=[B_in[i]])
                    dma("sp", Vt[i][:], rv_d.ap()[tok, :], pw=[B_in[i]])
                    dma("sp", Gt[i][:], rg_d.ap()[tok, :], pw=[B_in[i]])

                def chunkA(c):
                    i = c % 2
                    ii = c % 3
                    pout, B_pout = pout2[i], B_pout2[i]
                    for a in range(4):
                        op("pe", lambda a=a: T.transpose(out=pkt_t[:, a, :], in_=KT[ii][:, a, :], identity=identb[:]),
                           r=[B_in[ii], B_const], pw=[B_pkt])
                    op("act", lambda: A.copy(out=Kt[i][:].rearrange("p (a s) -> p a s", a=4), in_=pkt),
                       r=[B_pkt], w=[B_Kt[i]])
                    op("dve", lambda: V.tensor_tensor(out=Vd[i][:], in0=Vt[ii][:].rearrange("p (h e) -> p h e", h=NH),
                                                       in1=bc3(kdec, 64), op=ALU.mult),
                       r=[B_in[ii], B_c3], w=[B_Vd[i]])
                    def scores(h):
                        a, hf = h // 2, h % 2
                        prt = slice(64 * hf, 64 * hf + 64)
                        si = cnt["sc"] % 2
                        cnt["sc"] += 1
                        ai = cnt["at"] % 3
                        cnt["at"] += 1
                        op("pe", lambda: T.matmul(psc[si], lhsT=KT[ii][prt, a, :], rhs=QT[ii][prt, a, :],
                                                  start=True, stop=True), r=[B_in[ii]], w=[B_psc[si]])
                        op("dve", lambda: V.tensor_tensor(out=AT[ai][:], in0=psc[si], in1=DT[:, h, :], op=ALU.mult),
                           r=[B_psc[si], B_c3], w=[B_AT[ai]])
                        return ai

                    def inner(h, ai):
                        a, hf = h // 2, h % 2
                        prt = slice(64 * hf, 64 * hf + 64)
                        op("pe", lambda: T.matmul(pout[:, h, :], lhsT=AT[ai][:], rhs=Vt[ii][:, h * 64:(h + 1) * 64],
                                                  start=True, stop=False),
                           r=[B_AT[ai], B_in[ii]], pw=[B_pout], signal=False)
                        op("pe", lambda: T.matmul(pout[:, h, :], lhsT=QT[ii][prt, a, :], rhs=stb[prt, a, :],
                                                  start=False, stop=True),
                           r=[B_in[ii], B_stb], pw=[B_pout])

                    ais = [scores(0)]
                    for h in range(NH):
                        if h + 1 < NH:
                            ais.append(scores(h + 1))
                        inner(h, ais[h])
                    for h in range(NH):
                        a, hf = h // 2, h % 2
                        prt = slice(64 * hf, 64 * hf + 64)
                        op("pe", lambda: T.matmul(pstate_t[prt, a, :], lhsT=Kt[i][:, h * 64:(h + 1) * 64],
                                                  rhs=Vd[i][:, h, :], start=True, stop=True),
                           r=[B_Kt[i], B_Vd[i]], pw=[B_pstate], signal=(h == NH - 1))
                    op("dve", lambda: V.tensor_tensor(out=stf[:], in0=stf[:], in1=cdec[:], op=ALU.mult),
                       r=[B_c3], w=[B_stf])
                    op("dve", lambda: V.tensor_tensor(out=stf[:], in0=pstate, in1=stf[:], op=ALU.add),
                       r=[B_pstate], w=[B_stf])
                    op("act", lambda: A.copy(out=stb[:], in_=stf[:]), r=[B_stf], w=[B_stb])
                def chunkB(c):
                    i = c % 2
                    ii = c % 3
                    pout, B_pout = pout2[i], B_pout2[i]
                    op("dve", lambda: V.tensor_tensor(out=raw[:], in0=pout[:], in1=bc3(qdec, 64), op=ALU.mult),
                       r=[B_pout, B_c3], w=[B_raw])
                    op("dve", lambda: V.tensor_reduce(out=mean[:], in_=raw[:], op=ALU.add, axis=mybir.AxisListType.X),
                       r=[B_raw], w=[B_mean])
                    op("dve", lambda: V.tensor_scalar(out=mean[:], in0=mean[:], scalar1=1.0 / HD, scalar2=None,
                                                      op0=ALU.mult), r=[], w=[B_mean])
                    op("dve", lambda: V.tensor_tensor(out=cen[:], in0=raw[:], in1=bc3(mean, 64), op=ALU.subtract),
                       r=[B_raw, B_mean], w=[B_cen])
                    op("act", lambda: A.activation(out=sq[:], in_=cen[:], func=AF.Square),
                       r=[B_cen], w=[B_sq])
                    op("dve", lambda: V.tensor_reduce(out=var[:], in_=sq[:], op=ALU.add, axis=mybir.AxisListType.X),
                       r=[B_sq], w=[B_var])
                    op("act", lambda: A.activation(out=sdv[:], in_=var[:], func=AF.Sqrt, bias=epsg[:], scale=1.0 / HD),
                       r=[B_var, B_const], w=[B_sdv])
                    op("dve", lambda: V.reciprocal(out=rstd[:], in_=sdv[:]), r=[B_sdv], w=[B_rstd])
                    op("dve", lambda: V.tensor_tensor(out=nrm[:], in0=cen[:], in1=bc3(rstd, 64), op=ALU.mult),
                       r=[B_cen, B_rstd], w=[B_nrm])
                    op("act", lambda: A.activation(out=sg[:], in_=Gt[ii][:], func=AF.Silu), r=[B_in[ii]], w=[B_sg])
                    op("dve", lambda: V.tensor_tensor(out=res[i][:], in0=nrm[:].rearrange("p h e -> p (h e)"),
                                                       in1=sg[:], op=ALU.mult), r=[B_nrm, B_sg], w=[B_res[i]])
                    dma("pool", mixed_d.ap()[c * 128:(c + 1) * 128, 512:1024], res[i][:], r=[B_res[i]])

                load(0)
                if NB > 1:
                    load(1)
                chunkA(0)
                for c in range(NB):
                    if c + 2 < NB:
                        load(c + 2)
                    if c + 1 < NB:
                        chunkA(c + 1)
                    chunkB(c)
                sc.barrier(barr[:])

        def phase4a(l, hsrc):
            with contextlib.ExitStack() as st:
                wo = sbt(st, "wo", [128, 8, D], BF16)
                wu = sbt(st, "wu", [128, 8, 2 * DFF], BF16)
                B_wo, B_wu = Buf("wo"), Buf("wu")
                for k in range(8):
                    load_w(wo, k, w_out.ap()[l, k * 128:(k + 1) * 128, :], D, B_wo, 1024)
                for k in range(8):
                    load_w(wu, k, w_up.ap()[l, k * 128:(k + 1) * 128, :], 2 * DFF, B_wu, 1408)
                wn = sbt(st, "wn2", [128, D], F32)
                B_wn = Buf("wn")
                bcast_row(wn[:], ffn_norm_w, l * D, D, B_wn)
                nm = Normer(st, "n2", wn, B_wn)
                cwr = sbt(st, "cwr", [4 * NFC, 128], F32)
                cw = sbt(st, "cw", [128, 4 * NFC], F32)
                B_cwr, B_cw = Buf("cwr"), Buf("cw")
                for j in range(3):
                    dma("sp", cwr[j * NFC:(j + 1) * NFC, :],
                        bass.AP(conv_w, (l * 3 + j) * DFF, [[128, NFC], [1, 128]]), pw=[B_cwr])
                dma("sp", cwr[3 * NFC:4 * NFC, :], bass.AP(conv_b, l * DFF, [[128, NFC], [1, 128]]), pw=[B_cwr])
                pa = [pst(st, "pa%d" % i, [128, 512], F32) for i in range(2)]
                pg = [pst(st, "pg%d" % i, [128, 512], F32) for i in range(3)]
                B_pa = [Buf("pa") for _ in range(2)]
                B_pg = [Buf("pg") for _ in range(3)]
                po = [pst(st, "po4_%d" % i, [128, 512], F32) for i in range(1)]
                B_po = [Buf("po") for _ in range(1)]
                op("pe", lambda: T.transpose(out=po[0][:, 0:4 * NFC], in_=cwr[:], identity=identf[0:4 * NFC, 0:4 * NFC]),
                   r=[B_cwr, B_const], w=[B_po[0]])
                op("dve", lambda: V.tensor_copy(out=cw[:], in_=po[0][:, 0:4 * NFC]), r=[B_po[0]], w=[B_cw])
                hb = [sbt(st, "hb4_%d" % i, [128, D], F32) for i in range(4)]
                mb = [sbt(st, "mb%d" % i, [128, D], BF16) for i in range(4)]
                B_hb = [Buf("hb") for _ in range(4)]
                B_mb = [Buf("mb") for _ in range(4)]
                mT = [sbt(st, "mT%d" % i, [128, 8, 128], BF16) for i in range(2)]
                B_mT = [Buf("mT") for _ in range(2)]
                uT = [sbt(st, "uT4_%d" % i, [128, 8, 512], BF16) for i in range(2)]
                B_uT = [Buf("uT") for _ in range(2)]
                halo = sbt(st, "halo", [128, NFC, 2], F32)
                B_halo = [Buf("halo") for _ in range(NFC)]
                op("pool", lambda: P.memset(halo[:], 0.0), w=B_halo)
                ab = [sbt(st, "ab%d" % i, [128, 514], F32) for i in range(2)]
                yb = [sbt(st, "yb%d" % i, [128, 512], F32) for i in range(2)]
                gb = [sbt(st, "gb%d" % i, [128, 512], F32) for i in range(2)]
                ao = [sbt(st, "ao%d" % i, [128, 512], BF16) for i in range(3)]
                B_ab = [Buf("ab") for _ in range(2)]
                B_yb = [Buf("yb") for _ in range(2)]
                B_gb = [Buf("gb") for _ in range(2)]
                B_ao = [Buf("ao") for _ in range(3)]
                cnt = {"po": 0, "f": 0, "ao": 0}

                def pre_gen(t):
                    s = t % 2
                    for sub in range(4):
                        blk = t * 4 + sub
                        rows = slice(blk * 128, (blk + 1) * 128)
                        dma("sp", hb[sub][:], hsrc[rows, :], w=[B_hb[sub]])
                        dma("sp", mb[sub][:], mixed_d.ap()[rows, :], w=[B_mb[sub]])
                    ubs = {}

                    def stA(sub):
                        nm.transpose8(mb[sub], B_mb[sub], mT[sub % 2], B_mT[sub % 2], 0, 8)

                    def stB(sub):
                        blk = t * 4 + sub
                        i = sub
                        mi = sub % 2
                        rows = slice(blk * 128, (blk + 1) * 128)
                        for nh in range(2):
                            for k in range(8):
                                op("pe", lambda k=k: T.matmul(po[0][:], lhsT=mT[mi][:, k, :],
                                                              rhs=wo[:, k, nh * 512:(nh + 1) * 512],
                                                              start=(k == 0), stop=(k == 7)),
                                   r=[B_mT[mi], B_wo], pw=[B_po[0]], signal=(k == 7))
                            op("dve", lambda: V.tensor_tensor(out=hb[i][:, nh * 512:(nh + 1) * 512], in0=po[0][:],
                                                              in1=hb[i][:, nh * 512:(nh + 1) * 512], op=ALU.add),
                               r=[B_po[0]], w=[B_hb[i]])
                        dma("pool", hB.ap()[rows, :], hb[i][:], r=[B_hb[i]])
                        ubs[sub] = nm.run_a(hb[i][:], B_hb[i])

                    def stC(sub):
                        ub_, B_ub_ = ubs[sub]
                        nm.transpose8(ub_, B_ub_, uT[s], B_uT[s], sub * 128, 8)

                    order = [(stA, 0), (stB, 0), (stA, 1), (stC, 0), (stB, 1), (stA, 2), (stC, 1), (stB, 2),
                             (stA, 3), (stC, 2), (stB, 3), (stC, 3)]
                    for f, sub in order:
                        f(sub)
                        yield

                def pre_tile(t):
                    for _ in pre_gen(t):
                        pass

                def ffn_tile(t):
                    s = t % 2
                    tok = slice(t * 512, (t + 1) * 512)
                    gen = pre_gen(t + 1) if t + 1 < NT else None
                    for fc in range(NFC):
                        i = cnt["f"] % 2
                        gi3 = cnt["f"] % 3
                        cnt["f"] += 1
                        if gen is not None and (fc % 2 == 0 or fc == NFC - 1):
                            next(gen, None)
                        for k in range(8):
                            op("pe", lambda k=k: T.matmul(pa[i][:], lhsT=wu[:, k, fc * 128:(fc + 1) * 128],
                                                          rhs=uT[s][:, k, :], start=(k == 0), stop=(k == 7)),
                               r=[B_wu, B_uT[s]], pw=[B_pa[i]], signal=(k == 7))
                        for k in range(8):
                            op("pe", lambda k=k: T.matmul(pg[gi3][:], lhsT=wu[:, k, DFF + fc * 128:DFF + (fc + 1) * 128],
                                                          rhs=uT[s][:, k, :], start=(k == 0), stop=(k == 7)),
                               r=[B_wu, B_uT[s]], pw=[B_pg[gi3]], signal=(k == 7))
                        a_, y_, g_ = ab[i], yb[i], gb[i]
                        op("act", lambda: A.copy(out=a_[:, 0:2], in_=halo[:, fc, :]), r=[B_halo[fc]], w=[B_ab[i]])
                        op("act", lambda: A.copy(out=a_[:, 2:514], in_=pa[i][:]), r=[B_pa[i]], pw=[B_ab[i]])
                        op("act", lambda: A.copy(out=halo[:, fc, :], in_=a_[:, 512:514]),
                           r=[B_ab[i]], w=[B_halo[fc]])
                        op("dve", lambda: V.tensor_scalar(out=y_[:], in0=a_[:, 2:514],
                                                          scalar1=cw[:, 2 * NFC + fc:2 * NFC + fc + 1],
                                                          scalar2=cw[:, 3 * NFC + fc:3 * NFC + fc + 1],
                                                          op0=ALU.mult, op1=ALU.add),
                           r=[B_ab[i], B_cw], w=[B_yb[i]])
                        op("dve", lambda: V.scalar_tensor_tensor(out=y_[:], in0=a_[:, 1:513],
                                                                 scalar=cw[:, NFC + fc:NFC + fc + 1], in1=y_[:],
                                                                 op0=ALU.mult, op1=ALU.add),
                           r=[B_ab[i], B_cw], w=[B_yb[i]])
                        op("dve", lambda: V.scalar_tensor_tensor(out=y_[:], in0=a_[:, 0:512],
                                                                 scalar=cw[:, fc:fc + 1], in1=y_[:],
                                                                 op0=ALU.mult, op1=ALU.add),
                           r=[B_ab[i], B_cw], w=[B_yb[i]])
                        op("act", lambda: A.activation(out=g_[:], in_=y_[:], func=AF.Gelu), r=[B_yb[i]], w=[B_gb[i]])
                        oi = cnt["ao"] % 3
                        cnt["ao"] += 1
                        op("dve", lambda: V.tensor_tensor(out=ao[oi][:], in0=pg[gi3][:], in1=g_[:], op=ALU.mult),
                           r=[B_pg[gi3], B_gb[i]], w=[B_ao[oi]])
                        dma("pool", act_d.ap()[fc * 128:(fc + 1) * 128, tok], ao[oi][:], r=[B_ao[oi]])
                    if gen is not None:
                        for _ in gen:
                            pass

                pre_tile(0)
                for t in range(NT):
                    ffn_tile(t)
                sc.barrier(barr[:])

        def phase4b(l, last):
            with contextlib.ExitStack() as st:
                wd = sbt(st, "wd", [128, NFC, D], BF16)
                wg = sbt(st, "wg", [128, 8, D], BF16)
                wp = sbt(st, "wp", [128, 2, D], BF16)
                B_wd, B_wg, B_wp = Buf("wd"), Buf("wg"), Buf("wp")
                for k in range(NFC):
                    load_w(wd, k, w_down.ap()[l, k * 128:(k + 1) * 128, :], D, B_wd, 1024)
                for k in range(8):
                    load_w(wg, k, w_ple_gate.ap()[l, k * 128:(k + 1) * 128, :], D, B_wg, 1024)
                for k in range(2):
                    load_w(wp, k, w_ple_proj.ap()[l, k * 128:(k + 1) * 128, :], D, B_wp, 1024)
                wn = sbt(st, "wn3", [128, D], F32)
                B_wn = Buf("wn")
                bcast_row(wn[:], ple_norm_w, l * D, D, B_wn)
                nm = Normer(st, "n3", wn, B_wn)
                if last:
                    fw = sbt(st, "fw", [128, D], F32)
                    B_fw = Buf("fw")
                    bcast_row(fw[:], final_norm_w, 0, D, B_fw)
                    ot = [sbt(st, "ot%d" % i, [128, D], F32) for i in range(2)]
                    B_ot = [Buf("ot") for _ in range(2)]
                aT = [sbt(st, "aT%d" % i, [128, NFC, 512], BF16) for i in range(2)]
                B_aT = [Buf("aT") for _ in range(2)]
                hb = [sbt(st, "hb5_%d" % i, [128, D], F32) for i in range(3)]
                B_hb = [Buf("hb") for _ in range(3)]
                pf32 = [sbt(st, "pf32_%d" % i, [128, PLE], F32) for i in range(2)]
                pb = [sbt(st, "pb%d" % i, [128, PLE], BF16) for i in range(2)]
                B_pf32 = [Buf("pf32") for _ in range(2)]
                B_pb = [Buf("pb") for _ in range(2)]
                u3T = [sbt(st, "u3T%d" % i, [128, 8, 128], BF16) for i in range(2)]
                pT = [sbt(st, "pT%d" % i, [128, 2, 128], BF16) for i in range(2)]
                B_u3T = [Buf("u3T") for _ in range(2)]
                B_pT = [Buf("pT") for _ in range(2)]
                gate = [sbt(st, "gate%d" % i, [128, 512], F32) for i in range(2)]
                tmp = [sbt(st, "tmp%d" % i, [128, 512], F32) for i in range(2)]
                B_gate = [Buf("gate") for _ in range(2)]
                B_tmp = [Buf("tmp") for _ in range(2)]
                pd = [pst(st, "pd%d" % i, [128, 512], F32) for i in range(2)]
                B_pd = [Buf("pd") for _ in range(2)]
                pgt = [pst(st, "pgt%d" % i, [128, 512], F32) for i in range(2)]
                B_pgt = [Buf("pgt") for _ in range(2)]
                ppp = [pst(st, "ppp%d" % i, [128, 512], F32) for i in range(2)]
                B_ppp = [Buf("ppp") for _ in range(2)]
                cnt = {"pd": 0, "g": 0}

                def load_tile(t):
                    s = t % 2
                    src = act_d.ap()[:, t * 512:(t + 1) * 512].rearrange("(c p) s -> p c s", p=128)
                    dma("sp", aT[s][:], src, w=[B_aT[s]])

                def down(blk):
                    t, sub = blk // 4, blk % 4
                    s = t % 2
                    i = blk % 2
                    hi = blk % 3
                    rows = slice(blk * 128, (blk + 1) * 128)
                    dma("sp", hb[hi][:], hB.ap()[rows, :], w=[B_hb[hi]])
                    dma("sp", pf32[i][:], p_in.ap()[l, rows, :], w=[B_pf32[i]])
                    for nh in range(2):
                        pi = cnt["pd"] % 2
                        cnt["pd"] += 1
                        for fc in range(NFC):
                            op("pe", lambda fc=fc: T.matmul(pd[pi][:], lhsT=aT[s][:, fc, sub * 128:(sub + 1) * 128],
                                                            rhs=wd[:, fc, nh * 512:(nh + 1) * 512],
                                                            start=(fc == 0), stop=(fc == NFC - 1)),
                               r=[B_aT[s], B_wd], pw=[B_pd[pi]], signal=(fc == NFC - 1))
                        op("dve", lambda: V.tensor_tensor(out=hb[hi][:, nh * 512:(nh + 1) * 512], in0=pd[pi][:],
                                                          in1=hb[hi][:, nh * 512:(nh + 1) * 512], op=ALU.add),
                           r=[B_pd[pi]], w=[B_hb[hi]])
                    ub_, B_ub_ = nm.run_a(hb[hi][:], B_hb[hi])
                    op("act", lambda: A.copy(out=pb[i][:], in_=pf32[i][:]), r=[B_pf32[i]], w=[B_pb[i]])
                    return ub_, B_ub_

                def rest(blk, ub_, B_ub_):
                    i = blk % 2
                    hi = blk % 3
                    rows = slice(blk * 128, (blk + 1) * 128)
                    nm.transpose8(ub_, B_ub_, u3T[i], B_u3T[i], 0, 8)
                    nm.transpose8(pb[i], B_pb[i], pT[i], B_pT[i], 0, 2)
                    for nh in range(2):
                        gi = cnt["g"] % 2
                        cnt["g"] += 1
                        cs = slice(nh * 512, (nh + 1) * 512)
                        for k in range(8):
                            op("pe", lambda k=k: T.matmul(pgt[gi][:], lhsT=u3T[i][:, k, :], rhs=wg[:, k, cs],
                                                          start=(k == 0), stop=(k == 7)),
                               r=[B_u3T[i], B_wg], pw=[B_pgt[gi]], signal=(k == 7))
                        for k in range(2):
                            op("pe", lambda k=k: T.matmul(ppp[gi][:], lhsT=pT[i][:, k, :], rhs=wp[:, k, cs],
                                                          start=(k == 0), stop=(k == 1)),
                               r=[B_pT[i], B_wp], pw=[B_ppp[gi]], signal=(k == 1))
                        op("act", lambda: A.activation(out=gate[gi][:], in_=pgt[gi][:], func=AF.Sigmoid),
                           r=[B_pgt[gi]], w=[B_gate[gi]])
                        op("dve", lambda: V.tensor_tensor(out=tmp[gi][:], in0=ppp[gi][:], in1=gate[gi][:],
                                                          op=ALU.mult), r=[B_ppp[gi], B_gate[gi]], w=[B_tmp[gi]])
                        op("dve", lambda: V.tensor_tensor(out=hb[hi][:, cs], in0=hb[hi][:, cs], in1=tmp[gi][:],
                                                          op=ALU.add), r=[B_tmp[gi]], w=[B_hb[hi]])
                    if last:
                        rs, B_rs = nm.rstd(hb[hi][:], B_hb[hi], 2)
                        oi = blk % 2
                        op("dve", lambda: V.scalar_tensor_tensor(out=ot[oi][:], in0=hb[hi][:], scalar=rs[:],
                                                                 in1=fw[:], op0=ALU.mult, op1=ALU.mult),
                           r=[B_hb[hi], B_rs, B_fw], w=[B_ot[oi]])
                        dma("pool", out.ap()[rows, :], ot[oi][:], r=[B_ot[oi]])
                    else:
                        dma("pool", hA.ap()[rows, :], hb[hi][:], r=[B_hb[hi]])

                load_tile(0)
                if NT > 1:
                    load_tile(1)
                nxt = down(0)
                for blk in range(NB):
                    cur = nxt
                    if blk + 1 < NB:
                        if (blk + 1) % 4 == 0 and (blk + 1) // 4 + 1 < NT:
                            load_tile((blk + 1) // 4 + 1)
                        nxt = down(blk + 1)
                    rest(blk, *cur)
                sc.barrier(barr[:])

        for l in range(depth):
            hsrc = x.ap() if l == 0 else hA.ap()
            if "1" in phases:
                phase1(l, hsrc)
            if "2" in phases:
                phase2(l)
            if "3" in phases:
                phase3(l)
            if "4" in phases:
                phase4a(l, hsrc)
            if "5" in phases:
                phase4b(l, do_final and l == depth - 1)
        print("sched: ops=%d waits=%d" % (sc.nops, sc.nwait), sc.cnt)
    return nc


def make_consts(S):
    bf = ml_dtypes.bfloat16
    inv_freq = (10000.0 ** (-np.arange(0, HD, 2, dtype=np.float32) / HD)).astype(np.float32)
    ang = (np.arange(S, dtype=np.float32)[:, None] * inv_freq[None, :]).astype(np.float32)
    cos = np.cos(ang).astype(np.float32).T
    sin = np.sin(ang).astype(np.float32).T
    cos128 = np.concatenate([cos, cos, cos, cos], axis=0)
    sin128 = np.concatenate([-sin, sin, -sin, sin], axis=0)
    pswap = np.zeros((128, 128), np.float32)
    for d in range(128):
        base = (d // 64) * 64
        dd = d % 64
        pswap[base + (dd + 32) % 64, d] = 1.0
    s_idx = np.arange(128)[:, None]
    t_idx = np.arange(128)[None, :]
    negmask = np.where(s_idx > t_idx, NEG, 0.0).astype(np.float32)
    lg = np.log1p(-np.exp2(-5.0 - np.arange(NH, dtype=np.float64)))
    j = np.arange(128, dtype=np.float64)
    DT = np.zeros((128, NH, 128), np.float64)
    for h in range(NH):
        DT[:, h, :] = np.where(s_idx <= t_idx, np.exp(-lg[h] * (j[:, None] + 1.0)), 0.0)
    qdec = np.exp(lg[None, :] * (j[:, None] + 1.0))
    kdec = np.exp(lg[None, :] * (127.0 - j[:, None]))
    cdec = np.zeros((128, 4, 64), np.float64)
    for a in range(4):
        cdec[:64, a, :] = np.exp(lg[2 * a] * 128.0)
        cdec[64:, a, :] = np.exp(lg[2 * a + 1] * 128.0)
    return {
        "c_cosq": np.ascontiguousarray(cos128), "c_sinq": np.ascontiguousarray(sin128),
        "c_cosk": np.ascontiguousarray(cos128 * np.float32(0.125)),
        "c_sink": np.ascontiguousarray(sin128 * np.float32(0.125)),
        "c_identb": np.eye(128, dtype=np.float32).astype(bf), "c_identf": np.eye(128, dtype=np.float32),
        "c_pswap": pswap.astype(bf), "c_negmask": negmask.astype(bf),
        "c_DT": DT.reshape(128, NH * 128).astype(np.float32), "c_qdec": qdec.astype(np.float32),
        "c_kdec": kdec.astype(np.float32), "c_cdec": cdec.reshape(128, 256).astype(np.float32),
    }


_WNAMES = ["attn_norm_w", "w_in", "forget_bias", "w_out", "ffn_norm_w", "w_up", "conv_w", "conv_b", "w_down",
           "ple_norm_w", "w_ple_gate", "w_ple_proj", "final_norm_w"]


def kernel(**inputs):
    x = np.asarray(inputs["x"], dtype=np.float32)
    p = np.asarray(inputs["p"], dtype=np.float32)
    B, S, _ = x.shape
    depth = p.shape[0]
    nc = build(S, depth)
    consts = make_consts(S)
    shared = {k: np.ascontiguousarray(np.asarray(inputs[k], dtype=np.float32)) for k in _WNAMES}
    shared.update(consts)
    in_maps = []
    for b in range(B):
        m = dict(shared)
        m["x"] = np.ascontiguousarray(x[b])
        m["p"] = np.ascontiguousarray(p[:, b])
        in_maps.append(m)
    res = run_bass_kernel_spmd(nc, in_maps, core_ids=list(range(B)))
    return np.stack([np.asarray(r["out"], dtype=np.float32) for r in res.results], axis=0)
```

```python
import contextlib
import os
import numpy as np
import ml_dtypes
import concourse.bass as bass
import concourse.mybir as mybir
from concourse.bass_utils import run_bass_kernel_spmd

F32 = mybir.dt.float32
BF16 = mybir.dt.bfloat16
AF = mybir.ActivationFunctionType
ALU = mybir.AluOpType

D = 1024
HD = 64
NH = 8
DFF = 2816
NFC = DFF // 128
PLE = 256
INW = 3592
C_FQ, C_FK, C_FV, C_FL, C_RQ, C_RK, C_RV, C_RG = 0, 512, 1024, 1536, 1544, 2056, 2568, 3080
NORM_EPS = 1e-6
GN_EPS = 1e-5
NEG = -30000.0


class Buf:
    __slots__ = ("name", "w", "r")

    def __init__(self, name):
        self.name = name
        self.w = {}
        self.r = {}


def _key(tok):
    return tok[1] if tok[0] == "e" else (tok[1], tok[2])


def _put(d, tok):
    k = _key(tok)
    o = d.get(k)
    if o is None or o[-1] < tok[-1]:
        d[k] = tok


class Sched:
    def __init__(self, nc, stack):
        self.nc = nc
        self.eng = {"pe": nc.tensor, "act": nc.scalar, "dve": nc.vector, "pool": nc.gpsimd, "sp": nc.sync}
        self.sem = {k: stack.enter_context(nc.semaphore("s_" + k)) for k in self.eng}
        self.cnt = {k: 0 for k in self.eng}
        self.waited = {k: {} for k in self.eng}
        self.dq = {}
        for q, n in (("sp", 16), ("pool", 24), ("act", 2)):
            self.dq[q] = [[stack.enter_context(nc.semaphore("d_%s%d" % (q, i))), 0] for i in range(n)]
        self.dqi = {q: 0 for q in self.dq}
        self.nwait = 0
        self.nops = 0

    def _wait(self, e, tok):
        if tok[0] == "e":
            p, v = tok[1], tok[2]
            if p == e and e == "pe":
                return
            key = p
            sem = self.sem[p]
        else:
            q, i, v = tok[1], tok[2], tok[3]
            key = (q, i)
            sem = self.dq[q][i][0]
        if self.waited[e].get(key, 0) >= v:
            return
        self.waited[e][key] = v
        self.eng[e].wait_ge(sem, v)
        self.nwait += 1

    def _deps(self, e, r, w, pw):
        toks = []
        for b in r:
            toks += list(b.w.values())
        for b in w:
            toks += list(b.w.values())
            toks += list(b.r.values())
        for b in pw:
            toks += list(b.r.values())
        for t in toks:
            self._wait(e, t)

    def _upd(self, tok, r, w, pw):
        for b in r:
            _put(b.r, tok)
        for b in w:
            b.w = {_key(tok): tok}
            b.r = {}
        for b in pw:
            if b.r:
                b.w = {_key(tok): tok}
                b.r = {}
            else:
                _put(b.w, tok)

    def op(self, e, fn, r=(), w=(), pw=(), signal=True):
        self._deps(e, r, w, pw)
        inst = fn()
        self.nops += 1
        if signal:
            self.cnt[e] += 1
            inst.then_inc(self.sem[e], 1)
            tok = ("e", e, self.cnt[e])
        else:
            tok = ("e", e, self.cnt[e] + 1)
        self._upd(tok, r, w, pw)
        return tok

    def dma(self, q, out, in_, r=(), w=(), pw=(), **kw):
        i = self.dqi[q]
        self.dqi[q] = (i + 1) % len(self.dq[q])
        slot = self.dq[q][i]
        if slot[1] > 0:
            self._wait(q, ("d", q, i, slot[1]))
        self._deps(q, r, w, pw)
        inst = self.eng[q].dma_start(out=out, in_=in_, **kw)
        self.nops += 1
        slot[1] += 16
        inst.then_inc(slot[0], 16)
        tok = ("d", q, i, slot[1])
        self._upd(tok, r, w, pw)
        return tok

    def barrier(self, scratch):
        m = "dve"
        for p in self.eng:
            if p != m and self.cnt[p] > 0:
                self._wait(m, ("e", p, self.cnt[p]))
        for q in self.dq:
            for i, (s, c) in enumerate(self.dq[q]):
                if c > 0:
                    self._wait(m, ("d", q, i, c))
        if self.cnt[m] > 0:
            self._wait(m, ("e", m, self.cnt[m]))
        inst = self.nc.vector.memset(scratch, 0.0)
        self.cnt[m] += 1
        inst.then_inc(self.sem[m], 1)
        tok = ("e", m, self.cnt[m])
        for e in self.eng:
            if e != m:
                self._wait(e, tok)
            for p in self.eng:
                self.waited[e][p] = max(self.waited[e].get(p, 0), self.cnt[p] if p != m or e != m else self.cnt[p])
            for q in self.dq:
                for i, (s, c) in enumerate(self.dq[q]):
                    self.waited[e][(q, i)] = max(self.waited[e].get((q, i), 0), c)


def sb_ap(t, offset, pairs):
    return bass.AP(t, offset, [list(p) for p in pairs])


def build(S, depth, do_final=True, phases="12345"):
    NB = S // 128
    NT = S // 512
    nc = bass.Bass("TRN2", target_bir_lowering=False)

    def din(name, shape, dt=F32):
        return nc.dram_tensor(name, list(shape), dt, kind="ExternalInput")

    def dscr(name, shape, dt):
        return nc.dram_tensor(name, list(shape), dt, kind="Internal")

    x = din("x", [S, D])
    p_in = din("p", [depth, S, PLE])
    attn_norm_w = din("attn_norm_w", [depth, D])
    w_in = din("w_in", [depth, D, INW])
    forget_bias = din("forget_bias", [depth, NH])
    w_out = din("w_out", [depth, D, D])
    ffn_norm_w = din("ffn_norm_w", [depth, D])
    w_up = din("w_up", [depth, D, 2 * DFF])
    conv_w = din("conv_w", [depth, 3, DFF])
    conv_b = din("conv_b", [depth, DFF])
    w_down = din("w_down", [depth, DFF, D])
    ple_norm_w = din("ple_norm_w", [depth, D])
    w_ple_gate = din("w_ple_gate", [depth, D, D])
    w_ple_proj = din("w_ple_proj", [depth, PLE, D])
    final_norm_w = din("final_norm_w", [D])
    c_cosq = din("c_cosq", [128, S])
    c_sinq = din("c_sinq", [128, S])
    c_cosk = din("c_cosk", [128, S])
    c_sink = din("c_sink", [128, S])
    c_identb = din("c_identb", [128, 128], BF16)
    c_identf = din("c_identf", [128, 128])
    c_pswap = din("c_pswap", [128, 128], BF16)
    c_negmask = din("c_negmask", [128, 128], BF16)
    c_DT = din("c_DT", [128, NH * 128])
    c_qdec = din("c_qdec", [128, NH])
    c_kdec = din("c_kdec", [128, NH])
    c_cdec = din("c_cdec", [128, 4 * 64])

    out = nc.dram_tensor("out", [S, D], F32, kind="ExternalOutput")

    hA = dscr("hA", [S, D], F32)
    hB = dscr("hB", [S, D], F32)
    fqT_d = dscr("fqT_d", [NH, 65, S], BF16)
    fkT_d = dscr("fkT_d", [NH, 64, S], BF16)
    fv_d = dscr("fv_d", [S, 512], BF16)
    rqT_d = dscr("rqT_d", [512, S], BF16)
    rkT_d = dscr("rkT_d", [512, S], BF16)
    rv_d = dscr("rv_d", [S, 512], BF16)
    rg_d = dscr("rg_d", [S, 512], BF16)
    mixed_d = dscr("mixed_d", [S, 1024], BF16)
    act_d = dscr("act_d", [DFF, S], BF16)

    with contextlib.ExitStack() as gst:
        G = gst.enter_context
        sc = Sched(nc, gst)
        op, dma = sc.op, sc.dma
        V, A, P, T = nc.vector, nc.scalar, nc.gpsimd, nc.tensor

        uid = {"n": 0}

        def sbt(st, name, shape, dt):
            uid["n"] += 1
            return st.enter_context(nc.sbuf_tensor("%s_%d" % (name, uid["n"]), list(shape), dt))

        def pst(st, name, shape, dt):
            uid["n"] += 1
            return st.enter_context(nc.psum_tensor("%s_%d" % (name, uid["n"]), list(shape), dt))

        identb = sbt(gst, "identb", [128, 128], BF16)
        identf = sbt(gst, "identf", [128, 128], F32)
        pswap = sbt(gst, "pswap", [128, 128], BF16)
        negmask = sbt(gst, "negmask", [128, 128], BF16)
        epsn = sbt(gst, "epsn", [128, 1], F32)
        epsg = sbt(gst, "epsg", [128, 1], F32)
        barr = sbt(gst, "barr", [128, 1], F32)
        negcum = sbt(gst, "negcum", [128, NB, NH], F32)
        B_const = Buf("const")
        B_negcum = Buf("negcum")
        dma("sp", identb[:], c_identb.ap(), w=[B_const])
        dma("sp", identf[:], c_identf.ap(), pw=[B_const])
        dma("sp", pswap[:], c_pswap.ap(), pw=[B_const])
        dma("sp", negmask[:], c_negmask.ap(), pw=[B_const])
        op("pool", lambda: P.memset(epsn[:], NORM_EPS), pw=[B_const])
        op("pool", lambda: P.memset(epsg[:], GN_EPS), pw=[B_const])
        sc.barrier(barr[:])

        rr = {"i": 0}

        def evac_engine():
            rr["i"] += 1
            return "act" if rr["i"] % 2 else "dve"

        def copy_on(e, out_ap, in_ap, r, w=(), pw=(), scale=None):
            if e == "act":
                if scale is None:
                    return op("act", lambda: A.copy(out=out_ap, in_=in_ap), r=r, w=w, pw=pw)
                return op("act", lambda: A.mul(out=out_ap, in_=in_ap, mul=scale), r=r, w=w, pw=pw)
            if e == "dve":
                if scale is None:
                    return op("dve", lambda: V.tensor_copy(out=out_ap, in_=in_ap), r=r, w=w, pw=pw)
                return op("dve", lambda: V.tensor_scalar(out=out_ap, in0=in_ap, scalar1=scale, scalar2=None,
                                                         op0=ALU.mult), r=r, w=w, pw=pw)
            if scale is None:
                return op("pool", lambda: P.tensor_copy(out=out_ap, in_=in_ap), r=r, w=w, pw=pw)
            return op("pool", lambda: P.tensor_scalar(out=out_ap, in0=in_ap, scalar1=scale, scalar2=None,
                                                      op0=ALU.mult), r=r, w=w, pw=pw)

        def load_w(dst, k, src_rows_ap, ncols, B, piece):
            c0 = 0
            while c0 < ncols:
                c1 = min(ncols, c0 + piece)
                dma("pool", dst[:, k, c0:c1], src_rows_ap[:, c0:c1], pw=[B])
                c0 = c1

        def bcast_row(dst, src_t, off, n, B):
            src = bass.AP(src_t, off, [[0, 128], [1, n]])
            dma("pool", dst, src, w=[B])

        class Normer:
            def __init__(self, st, tag, wn, B_wn):
                self.wn, self.B_wn = wn, B_wn
                self.junk = sbt(st, tag + "junk", [128, D], BF16)
                self.B_junk = Buf("junk")
                self.ss = [sbt(st, tag + "ss%d" % i, [128, 1], F32) for i in range(3)]
                self.sd = [sbt(st, tag + "sd%d" % i, [128, 1], F32) for i in range(3)]
                self.rs = [sbt(st, tag + "rs%d" % i, [128, 1], F32) for i in range(3)]
                self.B_ss = [Buf("ss") for _ in range(3)]
                self.B_sd = [Buf("sd") for _ in range(3)]
                self.B_rs = [Buf("rs") for _ in range(3)]
                self.ub = [sbt(st, tag + "ub%d" % i, [128, D], BF16) for i in range(2)]
                self.B_ub = [Buf("ub") for _ in range(2)]
                self.ptr = [pst(st, tag + "ptr%d" % i, [128, 8, 128], BF16) for i in range(2)]
                self.B_ptr = [Buf("ptr0"), Buf("ptr1")]
                self.i = 0

            def rstd(self, hb, B_hb, i=None):
                if i is None:
                    i = self.i % 2
                ss, sd, rs = self.ss[i], self.sd[i], self.rs[i]
                op("act", lambda: A.activation(out=self.junk[:], in_=hb, func=AF.Square, accum_out=ss[:]),
                   r=[B_hb], w=[self.B_junk, self.B_ss[i]])
                op("act", lambda: A.activation(out=sd[:], in_=ss[:], func=AF.Sqrt, bias=epsn[:], scale=1.0 / D),
                   r=[self.B_ss[i], B_const], w=[self.B_sd[i]])
                op("dve", lambda: V.reciprocal(out=rs[:], in_=sd[:]), r=[self.B_sd[i]], w=[self.B_rs[i]])
                return rs, self.B_rs[i]

            def run_a(self, hb, B_hb):
                i = self.i % 2
                rs, B_rs = self.rstd(hb, B_hb)
                ub = self.ub[i]
                op("dve", lambda: V.scalar_tensor_tensor(out=ub[:], in0=hb, scalar=rs[:], in1=self.wn[:],
                                                         op0=ALU.mult, op1=ALU.mult),
                   r=[B_hb, B_rs, self.B_wn], w=[self.B_ub[i]])
                self.i += 1
                return ub, self.B_ub[i]

            def run(self, hb, B_hb, uT, B_uT, c0):
                ub, B_ub = self.run_a(hb, B_hb)
                self.transpose8(ub, B_ub, uT, B_uT, c0, 8)

            def transpose8(self, ub, B_ub, uT, B_uT, c0, nk):
                for j in range((nk + 3) // 4):
                    n = min(4, nk - 4 * j)
                    for kk in range(n):
                        k = 4 * j + kk
                        op("pe", lambda k=k, kk=kk, j=j: T.transpose(out=self.ptr[j][:, kk, :],
                                                                     in_=ub[:, k * 128:(k + 1) * 128],
                                                                     identity=identb[:]),
                           r=[B_ub, B_const], pw=[self.B_ptr[j]])
                    copy_on(evac_engine(), uT[:, 4 * j:4 * j + n, c0:c0 + 128], self.ptr[j][:, 0:n, :],
                            r=[self.B_ptr[j]], pw=[B_uT])

        def phase1(l, hsrc):
            with contextlib.ExitStack() as st:
                win = sbt(st, "win", [128, 8, INW], BF16)
                B_win = Buf("win")
                for k in range(8):
                    load_w(win, k, w_in.ap()[l, k * 128:(k + 1) * 128, :], INW, B_win, 1796)
                wn = sbt(st, "wn1", [128, D], F32)
                B_wn = Buf("wn")
                bcast_row(wn[:], attn_norm_w, l * D, D, B_wn)
                fb = sbt(st, "fb", [NH, 1], F32)
                nfb = sbt(st, "nfb", [NH, 1], F32)
                B_fb, B_nfb = Buf("fb"), Buf("nfb")
                dma("sp", fb[:], bass.AP(forget_bias, l * NH, [[1, NH], [1, 1]]), w=[B_fb])
                op("dve", lambda: V.tensor_scalar(out=nfb[:], in0=fb[:], scalar1=-1.0, scalar2=None, op0=ALU.mult),
                   r=[B_fb], w=[B_nfb])
                nm = Normer(st, "n1", wn, B_wn)
                hb = [sbt(st, "hb%d" % i, [128, D], F32) for i in range(4)]
                B_hb = [Buf("hb") for _ in range(4)]
                uT = [sbt(st, "uT%d" % i, [128, 8, 512], BF16) for i in range(2)]
                B_uT = [Buf("uT") for _ in range(2)]
                tabs = [[sbt(st, "tab%d_%d" % (i, j), [128, 512], F32) for j in range(4)] for i in range(2)]
                B_tabs = [Buf("tabs") for _ in range(2)]
                pf = [pst(st, "pf%d" % i, [128, 512], F32) for i in range(2)]
                B_pf = [Buf("pf") for _ in range(2)]
                pt = [pst(st, "pt%d" % i, [128, 512], F32) for i in range(2)]
                B_pt = [Buf("pt") for _ in range(2)]
                pr = pst(st, "pr", [128, 512], F32)
                B_pr = Buf("pr")
                plnt = pst(st, "plnt", [128, 512], F32)
                pl = plnt[0:NH, :]
                pnt = plnt[:, 0:4 * NH].rearrange("p (j h) -> p j h", j=4)
                B_pl = Buf("plnt")
                B_pnt = B_pl
                NST = 4
                stg = [sbt(st, "stg%d" % i, [128, 512], BF16) for i in range(NST)]
                B_stg = [Buf("stg") for _ in range(NST)]
                qb = [sbt(st, "qb%d" % i, [128, 512], BF16) for i in range(2)]
                B_qb = [Buf("qb") for _ in range(2)]
                t1 = [sbt(st, "t1_%d" % i, [128, 512], F32) for i in range(2)]
                t2 = [sbt(st, "t2_%d" % i, [128, 512], F32) for i in range(2)]
                B_t1 = [Buf("t1") for _ in range(2)]
                B_t2 = [Buf("t2") for _ in range(2)]
                ef = sbt(st, "ef", [NH, 512], F32)
                lf = sbt(st, "lf", [NH, 512], F32)
                B_ef, B_lf = Buf("ef"), Buf("lf")
                ones8 = sbt(st, "ones8", [NH, 512], F32)
                B_ones8 = Buf("ones8")
                op("pool", lambda: P.memset(ones8[:], 1.0), w=[B_ones8])
                ncT = [sbt(st, "ncT%d" % i, [NH, 512], F32) for i in range(2)]
                B_ncT = [Buf("ncT") for _ in range(2)]
                cb = sbt(st, "cb", [NH, 512], BF16)
                B_cb = Buf("cb")
                cnt = {"pf": 0, "pt": 0, "stg": 0, "rot": 0}

                def norm_gen(t):
                    s = t % 2
                    tb = tabs[s]
                    for j, c in enumerate((c_cosq, c_sinq, c_cosk, c_sink)):
                        dma("sp", tb[j][:], c.ap()[:, t * 512:(t + 1) * 512], pw=[B_tabs[s]])
                    for sub in range(4):
                        blk = t * 4 + sub
                        dma("sp", hb[sub][:], hsrc[blk * 128:(blk + 1) * 128, :], w=[B_hb[sub]])
                    for sub in range(4):
                        i = sub
                        ub_, B_ub_ = nm.run_a(hb[i][:], B_hb[i])
                        yield
                        nm.transpose8(ub_, B_ub_, uT[s], B_uT[s], sub * 128, 8)
                        yield

                def norm_tile(t):
                    for _ in norm_gen(t):
                        pass

                def fmajor(t, c0, M=128):
                    s = t % 2
                    i = cnt["pf"] % 2
                    cnt["pf"] += 1
                    for k in range(8):
                        op("pe", lambda k=k: T.matmul(pf[i][0:M, :], lhsT=win[:, k, c0:c0 + M], rhs=uT[s][:, k, :],
                                                      start=(k == 0), stop=(k == 7)),
                           r=[B_win, B_uT[s]], pw=[B_pf[i]], signal=(k == 7))
                    return pf[i], B_pf[i]

                def next_stg():
                    i = cnt["stg"] % NST
                    cnt["stg"] += 1
                    return stg[i], B_stg[i]

                def mm_tile(t, lvl=9):
                    s = t % 2
                    tok = slice(t * 512, (t + 1) * 512)
                    gen = norm_gen(t + 1) if t + 1 < NT else None
                    pc = {"n": 0}

                    def pull():
                        pc["n"] += 1
                        if gen is not None and pc["n"] % 3 == 0:
                            next(gen, None)
                    for grp, c0, dst, scale in (("fq", C_FQ, fqT_d, 0.125), ("fk", C_FK, fkT_d, None)):
                        for cc in range(4):
                            ps, B_ps = fmajor(t, c0 + cc * 128)
                            sg, B_sg = next_stg()
                            copy_on(evac_engine(), sg[:], ps[:], r=[B_ps], w=[B_sg], scale=scale)
                            for hh in range(2):
                                dma("pool", dst.ap()[2 * cc + hh, 0:64, tok], sg[hh * 64:(hh + 1) * 64, :], r=[B_sg])
                            pull()
                    if lvl < 3:
                        return
                    i = cnt["pf"] % 2
                    cnt["pf"] += 1
                    for k in range(8):
                        op("pe", lambda k=k: T.matmul(pl, lhsT=win[:, k, C_FL:C_FL + NH], rhs=uT[s][:, k, :],
                                                      start=(k == 0), stop=(k == 7)),
                           r=[B_win, B_uT[s]], w=([B_pl] if k == 0 else []), pw=([] if k == 0 else [B_pl]),
                           signal=(k == 7))
                    op("act", lambda: A.activation(out=ef[:], in_=pl, func=AF.Exp, bias=nfb[:], scale=-1.0),
                       r=[B_pl, B_nfb], w=[B_ef])
                    op("act", lambda: A.activation(out=lf[:], in_=ef[:], func=AF.Ln, bias=1.0, scale=1.0),
                       r=[B_ef], w=[B_lf])
                    ci = t % 2
                    init = 0.0 if t == 0 else ncT[1 - ci][:, 511:512]
                    op("dve", lambda: V.tensor_tensor_scan(out=ncT[ci][:], data0=ones8[:], data1=lf[:], initial=init,
                                                           op0=ALU.mult, op1=ALU.add),
                       r=[B_lf, B_ones8, B_ncT[1 - ci]], w=[B_ncT[ci]])
                    op("dve", lambda: V.tensor_scalar(out=cb[:], in0=ncT[ci][:], scalar1=-1.0, scalar2=None,
                                                      op0=ALU.mult), r=[B_ncT[ci]], w=[B_cb])
                    dma("pool", fqT_d.ap()[:, 64, tok], cb[:], r=[B_cb])
                    for j in range(4):
                        op("pe", lambda j=j: T.transpose(out=pnt[:, j, :], in_=ncT[ci][:, j * 128:(j + 1) * 128],
                                                         identity=identf[0:NH, 0:NH]),
                           r=[B_ncT[ci], B_const], w=([B_pnt] if j == 0 else []), pw=([] if j == 0 else [B_pnt]))
                    op("dve", lambda: V.tensor_copy(out=negcum[:, t * 4:(t + 1) * 4, :], in_=pnt),
                       r=[B_pnt], pw=[B_negcum])
                    if lvl < 4:
                        return
                    deferred = {"f": None}
                    for grp, c0, dst, tj in (("rq", C_RQ, rqT_d, 0), ("rk", C_RK, rkT_d, 2)):
                        for cc in range(4):
                            ps, B_ps = fmajor(t, c0 + cc * 128)
                            ri = cnt["rot"] % 2
                            cnt["rot"] += 1
                            op("act", lambda: A.copy(out=qb[ri][:], in_=ps[:]), r=[B_ps], w=[B_qb[ri]])
                            op("dve", lambda: V.tensor_tensor(out=t1[ri][:], in0=ps[:], in1=tabs[s][tj][:],
                                                              op=ALU.mult), r=[B_ps, B_tabs[s], B_qb[ri]], w=[B_t1[ri]])
                            if deferred["f"] is not None:
                                deferred["f"]()

                            def rot_rest(ri=ri, tj=tj, dst=dst, cc=cc):
                                op("pe", lambda: T.matmul(pr[:], lhsT=pswap[:], rhs=qb[ri][:], start=True, stop=True),
                                   r=[B_qb[ri], B_const], w=[B_pr])
                                op("dve", lambda: V.tensor_tensor(out=t2[ri][:], in0=pr[:], in1=tabs[s][tj + 1][:],
                                                                  op=ALU.mult), r=[B_pr, B_tabs[s]], w=[B_t2[ri]])
                                sg, B_sg = next_stg()
                                op("dve", lambda: V.tensor_tensor(out=sg[:], in0=t1[ri][:], in1=t2[ri][:], op=ALU.add),
                                   r=[B_t1[ri], B_t2[ri]], w=[B_sg])
                                dma("pool", dst.ap()[cc * 128:(cc + 1) * 128, tok], sg[:], r=[B_sg])
                                deferred["f"] = None
                            deferred["f"] = rot_rest
                            pull()
                    if lvl < 5:
                        return
                    for sub in range(4):
                        rows = slice(t * 512 + sub * 128, t * 512 + (sub + 1) * 128)
                        for c0, dst in ((C_FV, fv_d), (C_RV, rv_d), (C_RG, rg_d)):
                            i = cnt["pt"] % 2
                            cnt["pt"] += 1
                            for k in range(8):
                                op("pe", lambda k=k: T.matmul(pt[i][:], lhsT=uT[s][:, k, sub * 128:(sub + 1) * 128],
                                                              rhs=win[:, k, c0:c0 + 512], start=(k == 0), stop=(k == 7)),
                                   r=[B_win, B_uT[s]], pw=[B_pt[i]], signal=(k == 7))
                            if deferred["f"] is not None:
                                deferred["f"]()
                            sg, B_sg = next_stg()
                            copy_on(evac_engine(), sg[:], pt[i][:], r=[B_pt[i]], w=[B_sg])
                            dma("pool", dst.ap()[rows, :], sg[:], r=[B_sg])
                            pull()
                    if gen is not None:
                        for _ in gen:
                            pass

                norm_tile(0)
                for t in range(NT):
                    mm_tile(t)
                sc.barrier(barr[:])

        def phase2(l):
            with contextlib.ExitStack() as st:
                KT = [sbt(st, "KT%d" % i, [65, S], BF16) for i in range(2)]
                QT = [sbt(st, "QT%d" % i, [65, S], BF16) for i in range(2)]
                VA = [sbt(st, "VA%d" % i, [128, NB, 65], BF16) for i in range(2)]
                B_KT = [Buf("KT") for _ in range(2)]
                B_QT = [Buf("QT") for _ in range(2)]
                B_VA = [Buf("VA") for _ in range(2)]
                for i in range(2):
                    op("pool", lambda i=i: P.memset(KT[i][64:65, :], 1.0), pw=[B_KT[i]])
                    op("pool", lambda i=i: P.memset(VA[i][:, :, 64:65], 1.0), pw=[B_VA[i]])
                NPS = 3
                ps = [pst(st, "ps%d" % i, [128, 512], F32) for i in range(NPS)]
                B_ps = [Buf("ps") for _ in range(NPS)]
                poT = [pst(st, "poT%d" % i, [128, 512], F32) for i in range(2)]
                B_poT = [Buf("poT") for _ in range(2)]
                pfin = pst(st, "pfin", [128, 4, 128], F32)
                B_pfin = Buf("pfin")
                NPT = 4
                PT = [sbt(st, "PT%d" % i, [128, 512], BF16) for i in range(NPT)]
                B_PT = [Buf("PT") for _ in range(NPT)]
                oT = [sbt(st, "oT%d" % i, [65, 512], F32) for i in range(2)]
                B_oT = [Buf("oT") for _ in range(2)]
                rsum = [sbt(st, "rsum%d" % i, [128, 4], F32) for i in range(2)]
                B_rsum = [Buf("rsum") for _ in range(2)]
                fo = [sbt(st, "fo%d" % i, [128, 4, 64], BF16) for i in range(2)]
                B_fo = [Buf("fo") for _ in range(2)]
                cnt = {"ps": 0, "fo": 0}

                def load_head(h):
                    i = h % 2
                    dma("sp", KT[i][0:64, :], fkT_d.ap()[h], pw=[B_KT[i]], r=[])
                    dma("sp", QT[i][0:65, :], fqT_d.ap()[h], w=[B_QT[i]])
                    src = fv_d.ap()[:, h * 64:(h + 1) * 64].rearrange("(n p) e -> p n e", p=128)
                    dma("sp", VA[i][:, :, 0:64], src, pw=[B_VA[i]])

                def emit_qk(stp):
                    h, qt, kb = stp
                    i = h % 2
                    q0 = qt * 512
                    di = kb - 4 * qt
                    j0 = max(di, 0)
                    N = 512 - 128 * j0
                    pi = cnt["ps"] % NPS
                    cnt["ps"] += 1
                    diag = di >= 0
                    op("pe", lambda: T.matmul(ps[pi][:, 0:N], lhsT=KT[i][0:65, kb * 128:(kb + 1) * 128],
                                              rhs=QT[i][0:65, q0 + 128 * j0:q0 + 512], start=True, stop=not diag),
                       r=[B_KT[i], B_QT[i]], w=[B_ps[pi]], signal=not diag)
                    if diag:
                        op("pe", lambda: T.matmul(ps[pi][:, 0:128], lhsT=identb[:], rhs=negmask[:],
                                                  start=False, stop=True), r=[B_const], pw=[B_ps[pi]])
                    return pi

                def emit_rest(stp, pi, fi):
                    h, qt, kb = stp
                    i = h % 2
                    nkb = 4 * qt + 4
                    j0 = max(kb - 4 * qt, 0)
                    N = 512 - 128 * j0
                    ti = cnt["pt"] % NPT
                    cnt["pt"] += 1
                    op("act", lambda: A.activation(out=PT[ti][:, 0:N], in_=ps[pi][:, 0:N], func=AF.Exp,
                                                   bias=negcum[:, kb, h:h + 1], scale=1.0),
                       r=[B_ps[pi], B_negcum], w=[B_PT[ti]])
                    op("pe", lambda: T.matmul(poT[fi][0:65, 128 * j0:512], lhsT=VA[i][:, kb, :],
                                              rhs=PT[ti][:, 0:N], start=(kb == 0), stop=(kb == nkb - 1)),
                       r=[B_PT[ti], B_VA[i]], pw=[B_poT[fi]])

                def fin_a(fi):
                    op("dve", lambda: V.tensor_copy(out=oT[fi][:], in_=poT[fi][0:65, :]), r=[B_poT[fi]], w=[B_oT[fi]])

                def fin_b(h, qt, fi):
                    q0 = qt * 512
                    for j in range(4):
                        op("pe", lambda j=j: T.transpose(out=pfin[:, j, 0:65], in_=oT[fi][0:65, j * 128:(j + 1) * 128],
                                                         identity=identf[0:65, 0:65]),
                           r=[B_oT[fi], B_const], w=([B_pfin] if j == 0 else []), pw=([] if j == 0 else [B_pfin]))
                    op("dve", lambda: V.reciprocal(out=rsum[fi][:].rearrange("p (j o) -> p j o", o=1),
                                                   in_=pfin[:, :, 64:65]), r=[B_pfin], w=[B_rsum[fi]])
                    op("dve", lambda: V.tensor_tensor(out=fo[fi][:], in0=pfin[:, :, 0:64],
                                                      in1=sb_ap(rsum[fi], 0, [[4, 128], [1, 4], [0, 64]]),
                                                      op=ALU.mult),
                       r=[B_pfin, B_rsum[fi]], w=[B_fo[fi]])
                    dst = mixed_d.ap()[q0:q0 + 512, h * 64:(h + 1) * 64].rearrange("(j p) e -> p j e", p=128)
                    dma("pool", dst, fo[fi][:], r=[B_fo[fi]])

                cnt["pt"] = 0
                steps = [(h, qt, kb) for h in range(NH) for qt in range(NT) for kb in range(4 * qt + 4)]
                LOOK = 2
                load_head(0)
                pis = [emit_qk(steps[n]) for n in range(min(LOOK, len(steps)))]
                pending = []
                tile_no = 0
                for n, stp in enumerate(steps):
                    h, qt, kb = stp
                    if qt == 0 and kb == 0 and h + 1 < NH:
                        load_head(h + 1)
                    if n + LOOK < len(steps):
                        pis.append(emit_qk(steps[n + LOOK]))
                    fi = tile_no % 2
                    emit_rest(stp, pis[n], fi)
                    for pnd in pending:
                        pnd[0] -= 1
                    while pending and pending[0][0] <= 0:
                        _, ph, pq, pf_ = pending.pop(0)
                        fin_b(ph, pq, pf_)
                    if kb == 4 * qt + 3:
                        fin_a(fi)
                        pending.append([2, h, qt, fi])
                        tile_no += 1
                for _, ph, pq, pf_ in pending:
                    fin_b(ph, pq, pf_)
                sc.barrier(barr[:])

        def phase3(l):
            with contextlib.ExitStack() as st:
                DT = sbt(st, "DT", [128, NH, 128], F32)
                qdec = sbt(st, "qdec", [128, NH], F32)
                kdec = sbt(st, "kdec", [128, NH], F32)
                cdec = sbt(st, "cdec", [128, 4, 64], F32)
                B_c3 = Buf("c3")
                dma("sp", DT[:], c_DT.ap().rearrange("p (h i) -> p h i", h=NH), w=[B_c3])
                dma("sp", qdec[:], c_qdec.ap(), pw=[B_c3])
                dma("sp", kdec[:], c_kdec.ap(), pw=[B_c3])
                dma("sp", cdec[:], c_cdec.ap().rearrange("p (a e) -> p a e", a=4), pw=[B_c3])
                stf = sbt(st, "stf", [128, 4, 64], F32)
                stb = sbt(st, "stb", [128, 4, 64], BF16)
                B_stf, B_stb = Buf("stf"), Buf("stb")
                op("pool", lambda: P.memset(stf[:], 0.0), w=[B_stf])
                op("pool", lambda: P.memset(stb[:], 0.0), w=[B_stb])
                QT = [sbt(st, "rQT%d" % i, [128, 4, 128], BF16) for i in range(3)]
                KT = [sbt(st, "rKT%d" % i, [128, 4, 128], BF16) for i in range(3)]
                Vt = [sbt(st, "rV%d" % i, [128, 512], BF16) for i in range(3)]
                Gt = [sbt(st, "rG%d" % i, [128, 512], BF16) for i in range(3)]
                B_in = [Buf("r_in") for _ in range(3)]
                Kt = [sbt(st, "rKt%d" % i, [128, 512], BF16) for i in range(2)]
                B_Kt = [Buf("Kt") for _ in range(2)]
                Vd = [sbt(st, "rVd%d" % i, [128, NH, 64], BF16) for i in range(2)]
                B_Vd = [Buf("Vd") for _ in range(2)]
                AT = [sbt(st, "rAT%d" % i, [128, 128], BF16) for i in range(3)]
                B_AT = [Buf("AT") for _ in range(3)]
                pkt_t = pst(st, "pkt", [128, 8, 128], BF16)
                pkt = pkt_t[:, 0:4, :]
                B_pkt = Buf("pkt")
                psc_t = [pst(st, "psc%d" % i, [128, 512], F32) for i in range(2)]
                psc = [t_[:, 0:128] for t_ in psc_t]
                B_psc = [Buf("psc") for _ in range(2)]
                pout2 = [pst(st, "pout%d" % i, [128, NH, 64], F32) for i in range(2)]
                B_pout2 = [Buf("pout") for _ in range(2)]
                pstate_t = pst(st, "pstate", [128, 8, 64], F32)
                pstate = pstate_t[:, 0:4, :]
                B_pstate = Buf("pstate")
                raw = sbt(st, "raw", [128, NH, 64], F32)
                cen = sbt(st, "cen", [128, NH, 64], F32)
                sq = sbt(st, "sq", [128, NH, 64], F32)
                nrm = sbt(st, "nrm", [128, NH, 64], F32)
                sg = sbt(st, "sg", [128, 512], F32)
                B_raw, B_cen, B_sq, B_nrm, B_sg = Buf("raw"), Buf("cen"), Buf("sq"), Buf("nrm"), Buf("sg")
                mean = sbt(st, "mean", [128, NH], F32)
                var = sbt(st, "var", [128, NH], F32)
                sdv = sbt(st, "sdv", [128, NH], F32)
                rstd = sbt(st, "rstd", [128, NH], F32)
                B_mean, B_var, B_sdv, B_rstd = Buf("mean"), Buf("var"), Buf("sdv"), Buf("rstd")
                res = [sbt(st, "res%d" % i, [128, 512], BF16) for i in range(2)]
                B_res = [Buf("res") for _ in range(2)]
                cnt = {"sc": 0, "at": 0}

                def bc3(t, n):
                    return sb_ap(t, 0, [[NH, 128], [1, NH], [0, n]])

                def load(c):
                    i = c % 3
                    tok = slice(c * 128, (c + 1) * 128)
                    dma("sp", QT[i][:], rqT_d.ap()[:, tok].rearrange("(a p) s -> p a s", p=128), w=[B_in[i]])
                    dma("sp", KT[i][:], rkT_d.ap()[:, tok].rearrange("(a p) s -> p a s", p=128), pw=[B_in[i]])
                    dma("sp", Vt[i][:], rv_d.ap()[tok, :], pw=[B_in[i]])
                    dma("sp", Gt[i][:], rg_d.ap()[tok, :], pw=[B_in[i]])

                def chunkA(c, genB=None):
                    i = c % 2
                    ii = c % 3
                    pout, B_pout = pout2[i], B_pout2[i]
                    for a in range(4):
                        op("pe", lambda a=a: T.transpose(out=pkt_t[:, a, :], in_=KT[ii][:, a, :], identity=identb[:]),
                           r=[B_in[ii], B_const], pw=[B_pkt])
                    op("act", lambda: A.copy(out=Kt[i][:].rearrange("p (a s) -> p a s", a=4), in_=pkt),
                       r=[B_pkt], w=[B_Kt[i]])
                    op("dve", lambda: V.tensor_tensor(out=Vd[i][:], in0=Vt[ii][:].rearrange("p (h e) -> p h e", h=NH),
                                                       in1=bc3(kdec, 64), op=ALU.mult),
                       r=[B_in[ii], B_c3], w=[B_Vd[i]])
                    def scores(h):
                        a, hf = h // 2, h % 2
                        prt = slice(64 * hf, 64 * hf + 64)
                        si = cnt["sc"] % 2
                        cnt["sc"] += 1
                        ai = cnt["at"] % 3
                        cnt["at"] += 1
                        op("pe", lambda: T.matmul(psc[si], lhsT=KT[ii][prt, a, :], rhs=QT[ii][prt, a, :],
                                                  start=True, stop=True), r=[B_in[ii]], w=[B_psc[si]])
                        op("dve", lambda: V.tensor_tensor(out=AT[ai][:], in0=psc[si], in1=DT[:, h, :], op=ALU.mult),
                           r=[B_psc[si], B_c3], w=[B_AT[ai]])
                        return ai

                    def inner(h, ai):
                        a, hf = h // 2, h % 2
                        prt = slice(64 * hf, 64 * hf + 64)
                        op("pe", lambda: T.matmul(pout[:, h, :], lhsT=AT[ai][:], rhs=Vt[ii][:, h * 64:(h + 1) * 64],
                                                  start=True, stop=False),
                           r=[B_AT[ai], B_in[ii]], pw=[B_pout], signal=False)
                        op("pe", lambda: T.matmul(pout[:, h, :], lhsT=QT[ii][prt, a, :], rhs=stb[prt, a, :],
                                                  start=False, stop=True),
                           r=[B_in[ii], B_stb], pw=[B_pout])

                    ais = [scores(0)]
                    for h in range(NH):
                        if h + 1 < NH:
                            ais.append(scores(h + 1))
                        inner(h, ais[h])
                        if genB is not None:
                            next(genB, None)
                            next(genB, None)
                    for h in range(NH):
                        a, hf = h // 2, h % 2
                        prt = slice(64 * hf, 64 * hf + 64)
                        op("pe", lambda: T.matmul(pstate_t[prt, a, :], lhsT=Kt[i][:, h * 64:(h + 1) * 64],
                                                  rhs=Vd[i][:, h, :], start=True, stop=True),
                           r=[B_Kt[i], B_Vd[i]], pw=[B_pstate], signal=(h == NH - 1))
                    op("dve", lambda: V.tensor_tensor(out=stf[:], in0=stf[:], in1=cdec[:], op=ALU.mult),
                       r=[B_c3], w=[B_stf])
                    op("dve", lambda: V.tensor_tensor(out=stf[:], in0=pstate, in1=stf[:], op=ALU.add),
                       r=[B_pstate], w=[B_stf])
                    op("act", lambda: A.copy(out=stb[:], in_=stf[:]), r=[B_stf], w=[B_stb])
                def chunkB(c):
                    i = c % 2
                    ii = c % 3
                    pout, B_pout = pout2[i], B_pout2[i]
                    op("dve", lambda: V.tensor_tensor(out=raw[:], in0=pout[:], in1=bc3(qdec, 64), op=ALU.mult),
                       r=[B_pout, B_c3], w=[B_raw])
                    yield
                    op("dve", lambda: V.tensor_reduce(out=mean[:], in_=raw[:], op=ALU.add, axis=mybir.AxisListType.X),
                       r=[B_raw], w=[B_mean])
                    yield
                    op("dve", lambda: V.tensor_scalar(out=mean[:], in0=mean[:], scalar1=1.0 / HD, scalar2=None,
                                                      op0=ALU.mult), r=[], w=[B_mean])
                    yield
                    op("dve", lambda: V.tensor_tensor(out=cen[:], in0=raw[:], in1=bc3(mean, 64), op=ALU.subtract),
                       r=[B_raw, B_mean], w=[B_cen])
                    yield
                    op("act", lambda: A.activation(out=sq[:], in_=cen[:], func=AF.Square),
                       r=[B_cen], w=[B_sq])
                    yield
                    op("dve", lambda: V.tensor_reduce(out=var[:], in_=sq[:], op=ALU.add, axis=mybir.AxisListType.X),
                       r=[B_sq], w=[B_var])
                    yield
                    op("act", lambda: A.activation(out=sdv[:], in_=var[:], func=AF.Sqrt, bias=epsg[:], scale=1.0 / HD),
                       r=[B_var, B_const], w=[B_sdv])
                    yield
                    op("dve", lambda: V.reciprocal(out=rstd[:], in_=sdv[:]), r=[B_sdv], w=[B_rstd])
                    yield
                    op("dve", lambda: V.tensor_tensor(out=nrm[:], in0=cen[:], in1=bc3(rstd, 64), op=ALU.mult),
                       r=[B_cen, B_rstd], w=[B_nrm])
                    yield
                    op("act", lambda: A.activation(out=sg[:], in_=Gt[ii][:], func=AF.Silu), r=[B_in[ii]], w=[B_sg])
                    yield
                    op("dve", lambda: V.tensor_tensor(out=res[i][:], in0=nrm[:].rearrange("p h e -> p (h e)"),
                                                       in1=sg[:], op=ALU.mult), r=[B_nrm, B_sg], w=[B_res[i]])
                    yield
                    dma("pool", mixed_d.ap()[c * 128:(c + 1) * 128, 512:1024], res[i][:], r=[B_res[i]])

                load(0)
                if NB > 1:
                    load(1)
                chunkA(0)
                for c in range(NB):
                    if c + 2 < NB:
                        load(c + 2)
                    gB = chunkB(c)
                    if c + 1 < NB:
                        chunkA(c + 1, gB)
                    for _ in gB:
                        pass
                sc.barrier(barr[:])

        def phase4a(l, hsrc):
            with contextlib.ExitStack() as st:
                wo = sbt(st, "wo", [128, 8, D], BF16)
                wu = sbt(st, "wu", [128, 8, 2 * DFF], BF16)
                B_wo, B_wu = Buf("wo"), Buf("wu")
                for k in range(8):
                    load_w(wo, k, w_out.ap()[l, k * 128:(k + 1) * 128, :], D, B_wo, 1024)
                for k in range(8):
                    load_w(wu, k, w_up.ap()[l, k * 128:(k + 1) * 128, :], 2 * DFF, B_wu, 1408)
                wn = sbt(st, "wn2", [128, D], F32)
                B_wn = Buf("wn")
                bcast_row(wn[:], ffn_norm_w, l * D, D, B_wn)
                nm = Normer(st, "n2", wn, B_wn)
                cwr = sbt(st, "cwr", [4 * NFC, 128], F32)
                cw = sbt(st, "cw", [128, 4 * NFC], F32)
                B_cwr, B_cw = Buf("cwr"), Buf("cw")
                for j in range(3):
                    dma("sp", cwr[j * NFC:(j + 1) * NFC, :],
                        bass.AP(conv_w, (l * 3 + j) * DFF, [[128, NFC], [1, 128]]), pw=[B_cwr])
                dma("sp", cwr[3 * NFC:4 * NFC, :], bass.AP(conv_b, l * DFF, [[128, NFC], [1, 128]]), pw=[B_cwr])
                pa = [pst(st, "pa%d" % i, [128, 512], F32) for i in range(2)]
                pg = [pst(st, "pg%d" % i, [128, 512], F32) for i in range(3)]
                B_pa = [Buf("pa") for _ in range(2)]
                B_pg = [Buf("pg") for _ in range(3)]
                po = [pst(st, "po4_%d" % i, [128, 512], F32) for i in range(1)]
                B_po = [Buf("po") for _ in range(1)]
                op("pe", lambda: T.transpose(out=po[0][:, 0:4 * NFC], in_=cwr[:], identity=identf[0:4 * NFC, 0:4 * NFC]),
                   r=[B_cwr, B_const], w=[B_po[0]])
                op("dve", lambda: V.tensor_copy(out=cw[:], in_=po[0][:, 0:4 * NFC]), r=[B_po[0]], w=[B_cw])
                hb = [sbt(st, "hb4_%d" % i, [128, D], F32) for i in range(4)]
                mb = [sbt(st, "mb%d" % i, [128, D], BF16) for i in range(4)]
                B_hb = [Buf("hb") for _ in range(4)]
                B_mb = [Buf("mb") for _ in range(4)]
                mT = [sbt(st, "mT%d" % i, [128, 8, 128], BF16) for i in range(2)]
                B_mT = [Buf("mT") for _ in range(2)]
                uT = [sbt(st, "uT4_%d" % i, [128, 8, 512], BF16) for i in range(2)]
                B_uT = [Buf("uT") for _ in range(2)]
                halo = sbt(st, "halo", [128, NFC, 2], F32)
                B_halo = [Buf("halo") for _ in range(NFC)]
                op("pool", lambda: P.memset(halo[:], 0.0), w=B_halo)
                ab = [sbt(st, "ab%d" % i, [128, 514], F32) for i in range(2)]
                yb = [sbt(st, "yb%d" % i, [128, 512], F32) for i in range(2)]
                gb = [sbt(st, "gb%d" % i, [128, 512], F32) for i in range(2)]
                ao = [sbt(st, "ao%d" % i, [128, 512], BF16) for i in range(3)]
                B_ab = [Buf("ab") for _ in range(2)]
                B_yb = [Buf("yb") for _ in range(2)]
                B_gb = [Buf("gb") for _ in range(2)]
                B_ao = [Buf("ao") for _ in range(3)]
                cnt = {"po": 0, "f": 0, "ao": 0}

                def pre_gen(t):
                    s = t % 2
                    for sub in range(4):
                        blk = t * 4 + sub
                        rows = slice(blk * 128, (blk + 1) * 128)
                        dma("sp", hb[sub][:], hsrc[rows, :], w=[B_hb[sub]])
                        dma("sp", mb[sub][:], mixed_d.ap()[rows, :], w=[B_mb[sub]])
                    ubs = {}

                    def stA(sub):
                        nm.transpose8(mb[sub], B_mb[sub], mT[sub % 2], B_mT[sub % 2], 0, 8)

                    def stB(sub):
                        blk = t * 4 + sub
                        i = sub
                        mi = sub % 2
                        rows = slice(blk * 128, (blk + 1) * 128)
                        for nh in range(2):
                            for k in range(8):
                                op("pe", lambda k=k: T.matmul(po[0][:], lhsT=mT[mi][:, k, :],
                                                              rhs=wo[:, k, nh * 512:(nh + 1) * 512],
                                                              start=(k == 0), stop=(k == 7)),
                                   r=[B_mT[mi], B_wo], pw=[B_po[0]], signal=(k == 7))
                            op("dve", lambda: V.tensor_tensor(out=hb[i][:, nh * 512:(nh + 1) * 512], in0=po[0][:],
                                                              in1=hb[i][:, nh * 512:(nh + 1) * 512], op=ALU.add),
                               r=[B_po[0]], w=[B_hb[i]])
                        dma("pool", hB.ap()[rows, :], hb[i][:], r=[B_hb[i]])
                        ubs[sub] = nm.run_a(hb[i][:], B_hb[i])

                    def stC(sub):
                        ub_, B_ub_ = ubs[sub]
                        nm.transpose8(ub_, B_ub_, uT[s], B_uT[s], sub * 128, 8)

                    order = [(stA, 0), (stB, 0), (stA, 1), (stC, 0), (stB, 1), (stA, 2), (stC, 1), (stB, 2),
                             (stA, 3), (stC, 2), (stB, 3), (stC, 3)]
                    for f, sub in order:
                        f(sub)
                        yield

                def pre_tile(t):
                    for _ in pre_gen(t):
                        pass

                def ffn_tile(t):
                    s = t % 2
                    tok = slice(t * 512, (t + 1) * 512)
                    gen = pre_gen(t + 1) if t + 1 < NT else None
                    for fc in range(NFC):
                        i = cnt["f"] % 2
                        gi3 = cnt["f"] % 3
                        cnt["f"] += 1
                        if gen is not None and (fc % 2 == 0 or fc == NFC - 1):
                            next(gen, None)
                        for k in range(8):
                            op("pe", lambda k=k: T.matmul(pa[i][:], lhsT=wu[:, k, fc * 128:(fc + 1) * 128],
                                                          rhs=uT[s][:, k, :], start=(k == 0), stop=(k == 7)),
                               r=[B_wu, B_uT[s]], pw=[B_pa[i]], signal=(k == 7))
                        for k in range(8):
                            op("pe", lambda k=k: T.matmul(pg[gi3][:], lhsT=wu[:, k, DFF + fc * 128:DFF + (fc + 1) * 128],
                                                          rhs=uT[s][:, k, :], start=(k == 0), stop=(k == 7)),
                               r=[B_wu, B_uT[s]], pw=[B_pg[gi3]], signal=(k == 7))
                        a_, y_, g_ = ab[i], yb[i], gb[i]
                        op("act", lambda: A.copy(out=a_[:, 0:2], in_=halo[:, fc, :]), r=[B_halo[fc]], w=[B_ab[i]])
                        op("act", lambda: A.copy(out=a_[:, 2:514], in_=pa[i][:]), r=[B_pa[i]], pw=[B_ab[i]])
                        op("act", lambda: A.copy(out=halo[:, fc, :], in_=a_[:, 512:514]),
                           r=[B_ab[i]], w=[B_halo[fc]])
                        op("dve", lambda: V.tensor_scalar(out=y_[:], in0=a_[:, 2:514],
                                                          scalar1=cw[:, 2 * NFC + fc:2 * NFC + fc + 1],
                                                          scalar2=cw[:, 3 * NFC + fc:3 * NFC + fc + 1],
                                                          op0=ALU.mult, op1=ALU.add),
                           r=[B_ab[i], B_cw], w=[B_yb[i]])
                        op("dve", lambda: V.scalar_tensor_tensor(out=y_[:], in0=a_[:, 1:513],
                                                                 scalar=cw[:, NFC + fc:NFC + fc + 1], in1=y_[:],
                                                                 op0=ALU.mult, op1=ALU.add),
                           r=[B_ab[i], B_cw], w=[B_yb[i]])
                        op("dve", lambda: V.scalar_tensor_tensor(out=y_[:], in0=a_[:, 0:512],
                                                                 scalar=cw[:, fc:fc + 1], in1=y_[:],
                                                                 op0=ALU.mult, op1=ALU.add),
                           r=[B_ab[i], B_cw], w=[B_yb[i]])
                        op("act", lambda: A.activation(out=g_[:], in_=y_[:], func=AF.Gelu), r=[B_yb[i]], w=[B_gb[i]])
                        oi = cnt["ao"] % 3
                        cnt["ao"] += 1
                        op("dve", lambda: V.tensor_tensor(out=ao[oi][:], in0=pg[gi3][:], in1=g_[:], op=ALU.mult),
                           r=[B_pg[gi3], B_gb[i]], w=[B_ao[oi]])
                        dma("pool", act_d.ap()[fc * 128:(fc + 1) * 128, tok], ao[oi][:], r=[B_ao[oi]])
                    if gen is not None:
                        for _ in gen:
                            pass

                pre_tile(0)
                for t in range(NT):
                    ffn_tile(t)
                sc.barrier(barr[:])

        def phase4b(l, last):
            with contextlib.ExitStack() as st:
                wd = sbt(st, "wd", [128, NFC, D], BF16)
                wg = sbt(st, "wg", [128, 8, D], BF16)
                wp = sbt(st, "wp", [128, 2, D], BF16)
                B_wd, B_wg, B_wp = Buf("wd"), Buf("wg"), Buf("wp")
                for k in range(NFC):
                    load_w(wd, k, w_down.ap()[l, k * 128:(k + 1) * 128, :], D, B_wd, 1024)
                for k in range(8):
                    load_w(wg, k, w_ple_gate.ap()[l, k * 128:(k + 1) * 128, :], D, B_wg, 1024)
                for k in range(2):
                    load_w(wp, k, w_ple_proj.ap()[l, k * 128:(k + 1) * 128, :], D, B_wp, 1024)
                wn = sbt(st, "wn3", [128, D], F32)
                B_wn = Buf("wn")
                bcast_row(wn[:], ple_norm_w, l * D, D, B_wn)
                nm = Normer(st, "n3", wn, B_wn)
                if last:
                    fw = sbt(st, "fw", [128, D], F32)
                    B_fw = Buf("fw")
                    bcast_row(fw[:], final_norm_w, 0, D, B_fw)
                    ot = [sbt(st, "ot%d" % i, [128, D], F32) for i in range(2)]
                    B_ot = [Buf("ot") for _ in range(2)]
                aT = [sbt(st, "aT%d" % i, [128, NFC, 512], BF16) for i in range(2)]
                B_aT = [Buf("aT") for _ in range(2)]
                hb = [sbt(st, "hb5_%d" % i, [128, D], F32) for i in range(3)]
                B_hb = [Buf("hb") for _ in range(3)]
                pf32 = [sbt(st, "pf32_%d" % i, [128, PLE], F32) for i in range(2)]
                pb = [sbt(st, "pb%d" % i, [128, PLE], BF16) for i in range(2)]
                B_pf32 = [Buf("pf32") for _ in range(2)]
                B_pb = [Buf("pb") for _ in range(2)]
                u3T = [sbt(st, "u3T%d" % i, [128, 8, 128], BF16) for i in range(2)]
                pT = [sbt(st, "pT%d" % i, [128, 2, 128], BF16) for i in range(2)]
                B_u3T = [Buf("u3T") for _ in range(2)]
                B_pT = [Buf("pT") for _ in range(2)]
                gate = [sbt(st, "gate%d" % i, [128, 512], F32) for i in range(2)]
                tmp = [sbt(st, "tmp%d" % i, [128, 512], F32) for i in range(2)]
                B_gate = [Buf("gate") for _ in range(2)]
                B_tmp = [Buf("tmp") for _ in range(2)]
                pd = [pst(st, "pd%d" % i, [128, 512], F32) for i in range(2)]
                B_pd = [Buf("pd") for _ in range(2)]
                pgt = [pst(st, "pgt%d" % i, [128, 512], F32) for i in range(2)]
                B_pgt = [Buf("pgt") for _ in range(2)]
                ppp = [pst(st, "ppp%d" % i, [128, 512], F32) for i in range(2)]
                B_ppp = [Buf("ppp") for _ in range(2)]
                cnt = {"pd": 0, "g": 0}

                def load_tile(t):
                    s = t % 2
                    src = act_d.ap()[:, t * 512:(t + 1) * 512].rearrange("(c p) s -> p c s", p=128)
                    dma("sp", aT[s][:], src, w=[B_aT[s]])

                def down(blk):
                    t, sub = blk // 4, blk % 4
                    s = t % 2
                    i = blk % 2
                    hi = blk % 3
                    rows = slice(blk * 128, (blk + 1) * 128)
                    dma("sp", hb[hi][:], hB.ap()[rows, :], w=[B_hb[hi]])
                    dma("sp", pf32[i][:], p_in.ap()[l, rows, :], w=[B_pf32[i]])
                    for nh in range(2):
                        pi = cnt["pd"] % 2
                        cnt["pd"] += 1
                        for fc in range(NFC):
                            op("pe", lambda fc=fc: T.matmul(pd[pi][:], lhsT=aT[s][:, fc, sub * 128:(sub + 1) * 128],
                                                            rhs=wd[:, fc, nh * 512:(nh + 1) * 512],
                                                            start=(fc == 0), stop=(fc == NFC - 1)),
                               r=[B_aT[s], B_wd], pw=[B_pd[pi]], signal=(fc == NFC - 1))
                        op("dve", lambda: V.tensor_tensor(out=hb[hi][:, nh * 512:(nh + 1) * 512], in0=pd[pi][:],
                                                          in1=hb[hi][:, nh * 512:(nh + 1) * 512], op=ALU.add),
                           r=[B_pd[pi]], w=[B_hb[hi]])
                    ub_, B_ub_ = nm.run_a(hb[hi][:], B_hb[hi])
                    op("act", lambda: A.copy(out=pb[i][:], in_=pf32[i][:]), r=[B_pf32[i]], w=[B_pb[i]])
                    return ub_, B_ub_

                def rest(blk, ub_, B_ub_):
                    i = blk % 2
                    hi = blk % 3
                    rows = slice(blk * 128, (blk + 1) * 128)
                    nm.transpose8(ub_, B_ub_, u3T[i], B_u3T[i], 0, 8)
                    nm.transpose8(pb[i], B_pb[i], pT[i], B_pT[i], 0, 2)
                    for nh in range(2):
                        gi = cnt["g"] % 2
                        cnt["g"] += 1
                        cs = slice(nh * 512, (nh + 1) * 512)
                        for k in range(8):
                            op("pe", lambda k=k: T.matmul(pgt[gi][:], lhsT=u3T[i][:, k, :], rhs=wg[:, k, cs],
                                                          start=(k == 0), stop=(k == 7)),
                               r=[B_u3T[i], B_wg], pw=[B_pgt[gi]], signal=(k == 7))
                        for k in range(2):
                            op("pe", lambda k=k: T.matmul(ppp[gi][:], lhsT=pT[i][:, k, :], rhs=wp[:, k, cs],
                                                          start=(k == 0), stop=(k == 1)),
                               r=[B_pT[i], B_wp], pw=[B_ppp[gi]], signal=(k == 1))
                        op("act", lambda: A.activation(out=gate[gi][:], in_=pgt[gi][:], func=AF.Sigmoid),
                           r=[B_pgt[gi]], w=[B_gate[gi]])
                        op("dve", lambda: V.tensor_tensor(out=tmp[gi][:], in0=ppp[gi][:], in1=gate[gi][:],
                                                          op=ALU.mult), r=[B_ppp[gi], B_gate[gi]], w=[B_tmp[gi]])
                        op("dve", lambda: V.tensor_tensor(out=hb[hi][:, cs], in0=hb[hi][:, cs], in1=tmp[gi][:],
                                                          op=ALU.add), r=[B_tmp[gi]], w=[B_hb[hi]])
                    if last:
                        rs, B_rs = nm.rstd(hb[hi][:], B_hb[hi], 2)
                        oi = blk % 2
                        op("dve", lambda: V.scalar_tensor_tensor(out=ot[oi][:], in0=hb[hi][:], scalar=rs[:],
                                                                 in1=fw[:], op0=ALU.mult, op1=ALU.mult),
                           r=[B_hb[hi], B_rs, B_fw], w=[B_ot[oi]])
                        dma("pool", out.ap()[rows, :], ot[oi][:], r=[B_ot[oi]])
                    else:
                        dma("pool", hA.ap()[rows, :], hb[hi][:], r=[B_hb[hi]])

                load_tile(0)
                if NT > 1:
                    load_tile(1)
                nxt = down(0)
                for blk in range(NB):
                    cur = nxt
                    if blk + 1 < NB:
                        if (blk + 1) % 4 == 0 and (blk + 1) // 4 + 1 < NT:
                            load_tile((blk + 1) // 4 + 1)
                        nxt = down(blk + 1)
                    rest(blk, *cur)
                sc.barrier(barr[:])

        for l in range(depth):
            hsrc = x.ap() if l == 0 else hA.ap()
            if "1" in phases:
                phase1(l, hsrc)
            if "2" in phases:
                phase2(l)
            if "3" in phases:
                phase3(l)
            if "4" in phases:
                phase4a(l, hsrc)
            if "5" in phases:
                phase4b(l, do_final and l == depth - 1)
        print("sched: ops=%d waits=%d" % (sc.nops, sc.nwait), sc.cnt)
    return nc


def make_consts(S):
    bf = ml_dtypes.bfloat16
    inv_freq = (10000.0 ** (-np.arange(0, HD, 2, dtype=np.float32) / HD)).astype(np.float32)
    ang = (np.arange(S, dtype=np.float32)[:, None] * inv_freq[None, :]).astype(np.float32)
    cos = np.cos(ang).astype(np.float32).T
    sin = np.sin(ang).astype(np.float32).T
    cos128 = np.concatenate([cos, cos, cos, cos], axis=0)
    sin128 = np.concatenate([-sin, sin, -sin, sin], axis=0)
    pswap = np.zeros((128, 128), np.float32)
    for d in range(128):
        base = (d // 64) * 64
        dd = d % 64
        pswap[base + (dd + 32) % 64, d] = 1.0
    s_idx = np.arange(128)[:, None]
    t_idx = np.arange(128)[None, :]
    negmask = np.where(s_idx > t_idx, NEG, 0.0).astype(np.float32)
    lg = np.log1p(-np.exp2(-5.0 - np.arange(NH, dtype=np.float64)))
    j = np.arange(128, dtype=np.float64)
    DT = np.zeros((128, NH, 128), np.float64)
    for h in range(NH):
        DT[:, h, :] = np.where(s_idx <= t_idx, np.exp(-lg[h] * (j[:, None] + 1.0)), 0.0)
    qdec = np.exp(lg[None, :] * (j[:, None] + 1.0))
    kdec = np.exp(lg[None, :] * (127.0 - j[:, None]))
    cdec = np.zeros((128, 4, 64), np.float64)
    for a in range(4):
        cdec[:64, a, :] = np.exp(lg[2 * a] * 128.0)
        cdec[64:, a, :] = np.exp(lg[2 * a + 1] * 128.0)
    return {
        "c_cosq": np.ascontiguousarray(cos128), "c_sinq": np.ascontiguousarray(sin128),
        "c_cosk": np.ascontiguousarray(cos128 * np.float32(0.125)),
        "c_sink": np.ascontiguousarray(sin128 * np.float32(0.125)),
        "c_identb": np.eye(128, dtype=np.float32).astype(bf), "c_identf": np.eye(128, dtype=np.float32),
        "c_pswap": pswap.astype(bf), "c_negmask": negmask.astype(bf),
        "c_DT": DT.reshape(128, NH * 128).astype(np.float32), "c_qdec": qdec.astype(np.float32),
        "c_kdec": kdec.astype(np.float32), "c_cdec": cdec.reshape(128, 256).astype(np.float32),
    }


_WNAMES = ["attn_norm_w", "w_in", "forget_bias", "w_out", "ffn_norm_w", "w_up", "conv_w", "conv_b", "w_down",
           "ple_norm_w", "w_ple_gate", "w_ple_proj", "final_norm_w"]


def kernel(**inputs):
    x = np.asarray(inputs["x"], dtype=np.float32)
    p = np.asarray(inputs["p"], dtype=np.float32)
    B, S, _ = x.shape
    depth = p.shape[0]
    nc = build(S, depth)
    consts = make_consts(S)
    shared = {k: np.ascontiguousarray(np.asarray(inputs[k], dtype=np.float32)) for k in _WNAMES}
    shared.update(consts)
    in_maps = []
    for b in range(B):
        m = dict(shared)
        m["x"] = np.ascontiguousarray(x[b])
        m["p"] = np.ascontiguousarray(p[:, b])
        in_maps.append(m)
    res = run_bass_kernel_spmd(nc, in_maps, core_ids=list(range(B)))
    return np.stack([np.asarray(r["out"], dtype=np.float32) for r in res.results], axis=0)
```
